# Optimizing a Trainium2 kernel written in Bass

```python
import math
import jax
import jax.numpy as jnp
from jax import lax
import numpy as np

D_MODEL = 1024
BATCH = 8
SEQ = 4096
DEPTH = 2

GRID_W = 64
CTX_LEN = 256
ATTN_WIDTH = D_MODEL // 2
SSD_WIDTH = D_MODEL // 4
RG_WIDTH = D_MODEL // 4
MIX_WIDTH = ATTN_WIDTH + SSD_WIDTH + RG_WIDTH
HEAD_DIM = 64
ATTN_HEADS = ATTN_WIDTH // HEAD_DIM
KV_HEADS = 2
Q_PER_KV = ATTN_HEADS // KV_HEADS
KV_WIDTH = KV_HEADS * HEAD_DIM
AXIS_DIM = HEAD_DIM // 2
ROPE_THETA = 10000.0
Q_BLOCK = 128
SSD_HEAD_DIM = 64
SSD_HEADS = SSD_WIDTH // SSD_HEAD_DIM
SSD_GROUPS = 2
SSD_STATE = 64
SSD_BC_WIDTH = SSD_GROUPS * SSD_STATE
SSD_XBC_WIDTH = SSD_WIDTH + 2 * SSD_BC_WIDTH
SSD_CHUNK = 128
RG_BLOCKS = 4
RG_BLOCK_DIM = RG_WIDTH // RG_BLOCKS
RG_C = 8.0
CONV_W = 4
CONV_PAD_LO = (CONV_W - 1) // 2
CONV_PAD_HI = CONV_W // 2
N_EXPERTS = 16
EXPERT_FF = 2 * D_MODEL
EC_CAPACITY = 2
EPS = 1e-6
DEEPNORM_ALPHA = (2 * DEPTH) ** 0.25
DEEPNORM_BETA = (8 * DEPTH) ** -0.25
IN_SPLITS = (ATTN_WIDTH, KV_WIDTH, KV_WIDTH, SSD_XBC_WIDTH, SSD_WIDTH, 2 * SSD_HEADS, RG_WIDTH, RG_WIDTH)
IN_WIDTH = ATTN_WIDTH + 2 * KV_WIDTH + SSD_XBC_WIDTH + SSD_WIDTH + 2 * SSD_HEADS + 2 * RG_WIDTH

kernel_name = 'hybrid_attn_ssd_rglru_ec_moe_dit'

F32 = jnp.float32


def split_columns(t, sizes):
    out, start = [], 0
    for s in sizes:
        out.append(t[..., start:start + s])
        start += s
    return out


def layer_norm(x, w, b):
    xf = x.astype(F32)
    mu = jnp.mean(xf, axis=-1, keepdims=True)
    var = jnp.mean(jnp.square(xf - mu), axis=-1, keepdims=True)
    return ((xf - mu) * lax.rsqrt(var + EPS) * w + b).astype(x.dtype)


def rms_norm(x, w):
    xf = x.astype(F32)
    return (xf * lax.rsqrt(jnp.mean(xf * xf, axis=-1, keepdims=True) + EPS) * w).astype(x.dtype)


def dw_conv(x, w, b):
    y = lax.conv_general_dilated(x, w[:, None, :].astype(x.dtype), window_strides=(1,),
                                 padding=[(CONV_PAD_LO, CONV_PAD_HI)],
                                 dimension_numbers=('NWC', 'WIO', 'NWC'),
                                 feature_group_count=x.shape[-1])
    return y + b


def to_heads(t, n_heads):
    return t.reshape(t.shape[0], t.shape[1], n_heads, HEAD_DIM)


def axial_rope_tables(n):
    rows = n // GRID_W
    row = jnp.repeat(jnp.arange(rows, dtype=F32), GRID_W)
    col = jnp.tile(jnp.arange(GRID_W, dtype=F32), rows)
    inv_freq = ROPE_THETA ** (-jnp.arange(0, AXIS_DIM, 2, dtype=F32) / AXIS_DIM)
    ang_r = row[:, None] * inv_freq
    ang_c = col[:, None] * inv_freq
    return (jnp.cos(ang_r), jnp.sin(ang_r), jnp.cos(ang_c), jnp.sin(ang_c))


def _rotate_half(t, cos, sin):
    t1, t2 = jnp.split(t, 2, axis=-1)
    cos = cos[:, None, :]
    sin = sin[:, None, :]
    return jnp.concatenate([t1 * cos - t2 * sin, t2 * cos + t1 * sin], axis=-1)


def apply_axial_rope(t, rope):
    cos_r, sin_r, cos_c, sin_c = rope
    out = jnp.concatenate([_rotate_half(t[..., :AXIS_DIM], cos_r, sin_r),
                           _rotate_half(t[..., AXIS_DIM:], cos_c, sin_c)], axis=-1)
    return out.astype(t.dtype)


def attend(q, k, v):
    s = jnp.einsum('bqkgd,blkd->bkgql', q, k).astype(F32) * (HEAD_DIM ** -0.5)
    p = jax.nn.softmax(s, axis=-1).astype(v.dtype)
    return jnp.einsum('bkgql,blkd->bqkgd', p, v)


def blocked_attention(q, k, v):
    b, n = q.shape[0], q.shape[1]
    nblk = n // Q_BLOCK
    qb = q.reshape(b, nblk, Q_BLOCK, KV_HEADS, Q_PER_KV, HEAD_DIM).transpose(1, 0, 2, 3, 4, 5)
    o = lax.map(lambda blk: attend(blk, k, v), qb)
    return o.transpose(1, 0, 2, 3, 4, 5).reshape(b, n, ATTN_WIDTH)


def segsum_from_cumsum(cs):
    t = cs.shape[-1]
    diff = cs[..., :, None] - cs[..., None, :]
    return jnp.where(jnp.tril(jnp.ones((t, t), dtype=bool)), diff, -jnp.inf)


def ssd_scan(xh, dt, a, bh, ch, h0):
    b, l, h, p = xh.shape
    n = bh.shape[-1]
    nc = l // SSD_CHUNK
    xdt = (xh * dt[..., None]).reshape(b, nc, SSD_CHUNK, h, p)
    bc = bh.reshape(b, nc, SSD_CHUNK, h, n)
    cc = ch.reshape(b, nc, SSD_CHUNK, h, n)
    da = (dt * a).reshape(b, nc, SSD_CHUNK, h).transpose(0, 3, 1, 2)
    da_cs = jnp.cumsum(da, axis=-1)
    l_mat = jnp.exp(segsum_from_cumsum(da_cs))
    y_diag = jnp.einsum('bclhn,bcshn,bhcls,bcshp->bclhp', cc, bc, l_mat, xdt)
    decay_states = jnp.exp(da_cs[..., -1:] - da_cs)
    states = jnp.einsum('bclhn,bhcl,bclhp->bchpn', bc, decay_states, xdt)
    states = jnp.concatenate([h0[:, None].astype(states.dtype), states], axis=1)
    chunk_tot = jnp.pad(da_cs[..., -1], ((0, 0), (0, 0), (1, 0)))
    decay_chunk = jnp.exp(segsum_from_cumsum(jnp.cumsum(chunk_tot, axis=-1)))
    new_states = jnp.einsum('bhzc,bchpn->bzhpn', decay_chunk, states)
    states_in, h_final = new_states[:, :-1], new_states[:, -1]
    y_off = jnp.einsum('bclhn,bchpn,bhcl->bclhp', cc, states_in, jnp.exp(da_cs))
    return (y_diag + y_off).reshape(b, l, h, p), h_final


def ssd_stream(xbc, dt_raw, lp, h0_f, h0_b):
    b, n, _ = xbc.shape
    xbc = jax.nn.silu(dw_conv(xbc, lp['ssd_conv_w'], lp['ssd_conv_b']))
    xs, bs, cs = split_columns(xbc, (SSD_WIDTH, SSD_BC_WIDTH, SSD_BC_WIDTH))
    xh = xs.reshape(b, n, SSD_HEADS, SSD_HEAD_DIM)
    rep = SSD_HEADS // SSD_GROUPS
    bh = jnp.repeat(bs.reshape(b, n, SSD_GROUPS, SSD_STATE), rep, axis=2)
    ch = jnp.repeat(cs.reshape(b, n, SSD_GROUPS, SSD_STATE), rep, axis=2)
    dt = jax.nn.softplus(dt_raw.astype(F32).reshape(b, n, 2, SSD_HEADS) + lp['ssd_dt_bias'].astype(F32))
    a = -jnp.exp(lp['ssd_a_log'].astype(F32))
    flip = lambda t: jnp.flip(t, axis=1)
    y_f, s_f = ssd_scan(xh, dt[:, :, 0], a[0], bh, ch, h0_f)
    y_b, s_b = ssd_scan(flip(xh), flip(dt[:, :, 1]), a[1], flip(bh), flip(ch), h0_b)
    y = y_f + flip(y_b) + lp['ssd_d'][:, None] * xh
    return y.reshape(b, n, SSD_WIDTH).astype(xs.dtype), s_f, s_b


def linear_scan(a, u, h0):
    u = u.at[:, 0].add(a[:, 0] * h0)

    def combine(left, right):
        a_l, u_l = left
        a_r, u_r = right
        return a_l * a_r, a_r * u_l + u_r

    _, h = lax.associative_scan(combine, (a, u), axis=1)
    return h


def rglru_stream(u_raw, lp, h0_f, h0_b):
    b, n, _ = u_raw.shape
    u = dw_conv(u_raw, lp['rg_conv_w'], lp['rg_conv_b'])
    ub = u.reshape(b, n, RG_BLOCKS, RG_BLOCK_DIM)

    def block_gate(w, bias):
        pre = jnp.einsum('bnkd,jkde->jbnke', ub, w).reshape(2, b, n, RG_WIDTH) + bias[:, None, None, :]
        return jax.nn.sigmoid(pre.astype(F32))

    r = block_gate(lp['rg_wa'], lp['rg_ba'])
    i = block_gate(lp['rg_wx'], lp['rg_bx'])
    log_a = -RG_C * r * jax.nn.softplus(-lp['rg_lambda'].astype(F32))[:, None, None, :]
    a = jnp.exp(log_a)
    inp = jnp.sqrt(-jnp.expm1(2.0 * log_a)) * i * u.astype(F32)[None]
    h_f = linear_scan(a[0], inp[0], h0_f)
    h_b = jnp.flip(linear_scan(jnp.flip(a[1], axis=1), jnp.flip(inp[1], axis=1), h0_b), axis=1)
    return (h_f + h_b).astype(u.dtype), h_f[:, -1], h_b[:, 0]


def expert_choice_ffn(h, lp):
    b, n, d = h.shape
    cap = EC_CAPACITY * n // N_EXPERTS
    aff = jax.nn.softmax(jnp.einsum('bnd,de->ben', h, lp['w_router']).astype(F32), axis=1)
    gate, idx = lax.top_k(aff, cap)
    xs = jax.vmap(lambda hb, ib: hb[ib])(h, idx)
    hid = jax.nn.silu(jnp.einsum('becd,edf->becf', xs, lp['w_gate'])) * jnp.einsum('becd,edf->becf', xs, lp['w_up'])
    ys = jnp.einsum('becf,efd->becd', hid, lp['w_down']) * gate[..., None].astype(h.dtype)
    return jax.vmap(lambda ib, yb: jnp.zeros((n, d), yb.dtype).at[ib.reshape(-1)].add(yb.reshape(-1, d)))(idx, ys)


def hybrid_layer(x, ctx, mod_x, mod_c, rope, lp, last):
    b = x.shape[0]
    m = ctx.shape[1]
    sh1, sc1, g1, sh2, sc2, g2 = jnp.split(mod_x[:, None, :], 6, axis=-1)
    csh1, csc1, cg1, csh2, csc2, cg2 = jnp.split(mod_c, 6, axis=-1)

    hx = x * (1 + sc1) + sh1
    hc = ctx * (1 + csc1) + csh1
    qx, kx, vx, xbc_x, zx, dtx, ux, gx = split_columns(hx @ lp['w_in'], IN_SPLITS)
    qc, kc, vc, xbc_c, zc, dtc, uc, gc = split_columns(hc @ lp['w_in'], IN_SPLITS)

    k_ctx = rms_norm(to_heads(kc, KV_HEADS), lp['k_norm'])
    v_ctx = to_heads(vc, KV_HEADS)
    q_lat = apply_axial_rope(rms_norm(to_heads(qx, ATTN_HEADS), lp['q_norm']), rope)
    k_lat = apply_axial_rope(rms_norm(to_heads(kx, KV_HEADS), lp['k_norm']), rope)
    k_all = jnp.concatenate([k_ctx, k_lat], axis=1)
    v_all = jnp.concatenate([v_ctx, to_heads(vx, KV_HEADS)], axis=1)
    attn_x = blocked_attention(q_lat, k_all, v_all)

    s0 = jnp.zeros((b, SSD_HEADS, SSD_HEAD_DIM, SSD_STATE), F32)
    ssd_c, s_f, s_b = ssd_stream(xbc_c, dtc, lp, s0, s0)
    ssd_x, _, _ = ssd_stream(xbc_x, dtx, lp, s_f, s_b)

    r0 = jnp.zeros((b, RG_WIDTH), F32)
    rg_c, r_f, r_b = rglru_stream(uc, lp, r0, r0)
    rg_x, _, _ = rglru_stream(ux, lp, r_f, r_b)

    def merge(attn, ssd, z, rg, gate):
        groups = [rms_norm(attn, lp['attn_out_norm']),
                  rms_norm(ssd * jax.nn.silu(z), lp['ssd_norm']),
                  rms_norm(rg * jax.nn.gelu(gate), lp['rg_out_norm'])]
        return jnp.concatenate(groups, axis=-1) @ lp['w_out']

    x = layer_norm(DEEPNORM_ALPHA * x + g1 * merge(attn_x, ssd_x, zx, rg_x, gx), lp['ln1_w'], lp['ln1_b'])
    if not last:
        q_ctx = rms_norm(to_heads(qc, ATTN_HEADS), lp['q_norm']).reshape(b, m, KV_HEADS, Q_PER_KV, HEAD_DIM)
        attn_c = attend(q_ctx, k_ctx, v_ctx).reshape(b, m, ATTN_WIDTH)
        ctx = layer_norm(DEEPNORM_ALPHA * ctx + cg1 * merge(attn_c, ssd_c, zc, rg_c, gc), lp['ln1_w'], lp['ln1_b'])

    x = layer_norm(DEEPNORM_ALPHA * x + g2 * expert_choice_ffn(x * (1 + sc2) + sh2, lp), lp['ln2_w'], lp['ln2_b'])
    if not last:
        ctx = layer_norm(DEEPNORM_ALPHA * ctx + cg2 * expert_choice_ffn(ctx * (1 + csc2) + csh2, lp), lp['ln2_w'], lp['ln2_b'])
    return x, ctx


def setup_inputs(seed: int = 0) -> dict:
    key = jax.random.key(seed)
    keys = iter(jax.random.split(key, 48))
    nrm = lambda shape, scale: jax.random.normal(next(keys), shape, F32) * scale
    L, D = DEPTH, D_MODEL
    dt0 = jnp.exp(jax.random.uniform(next(keys), (L, 2, SSD_HEADS), F32, math.log(1e-3), math.log(1e-1)))
    a0 = jax.random.uniform(next(keys), (L, 2, RG_WIDTH), F32, 0.9, 0.999)
    return {
        'x': nrm((BATCH, SEQ, D), 1.0),
        'c': nrm((BATCH, D), 1.0),
        'ctx': nrm((BATCH, CTX_LEN, D), 1.0),
        'c_ctx': nrm((D,), 1.0),
        'w_mod': nrm((L, D, 6 * D), 0.5 * D ** -0.5),
        'b_mod': nrm((L, 6 * D), 0.02),
        'w_in': nrm((L, D, IN_WIDTH), D ** -0.5),
        'q_norm': 1.0 + nrm((L, HEAD_DIM), 0.02),
        'k_norm': 1.0 + nrm((L, HEAD_DIM), 0.02),
        'attn_out_norm': 1.0 + nrm((L, ATTN_WIDTH), 0.02),
        'ssd_conv_w': nrm((L, CONV_W, SSD_XBC_WIDTH), CONV_W ** -0.5),
        'ssd_conv_b': nrm((L, SSD_XBC_WIDTH), 0.02),
        'ssd_dt_bias': dt0 + jnp.log(-jnp.expm1(-dt0)),
        'ssd_a_log': jnp.log(jax.random.uniform(next(keys), (L, 2, SSD_HEADS), F32, 1.0, 16.0)),
        'ssd_d': 1.0 + nrm((L, SSD_HEADS), 0.02),
        'ssd_norm': 1.0 + nrm((L, SSD_WIDTH), 0.02),
        'rg_conv_w': nrm((L, CONV_W, RG_WIDTH), CONV_W ** -0.5),
        'rg_conv_b': nrm((L, RG_WIDTH), 0.02),
        'rg_wa': nrm((L, 2, RG_BLOCKS, RG_BLOCK_DIM, RG_BLOCK_DIM), RG_BLOCK_DIM ** -0.5),
        'rg_ba': nrm((L, 2, RG_WIDTH), 0.02),
        'rg_wx': nrm((L, 2, RG_BLOCKS, RG_BLOCK_DIM, RG_BLOCK_DIM), RG_BLOCK_DIM ** -0.5),
        'rg_bx': nrm((L, 2, RG_WIDTH), 0.02),
        'rg_lambda': jnp.log(a0) - jnp.log1p(-a0),
        'rg_out_norm': 1.0 + nrm((L, RG_WIDTH), 0.02),
        'w_out': nrm((L, MIX_WIDTH, D), DEEPNORM_BETA * MIX_WIDTH ** -0.5),
        'ln1_w': 1.0 + nrm((L, D), 0.02),
        'ln1_b': nrm((L, D), 0.02),
        'w_router': nrm((L, D, N_EXPERTS), D ** -0.5),
        'w_gate': nrm((L, N_EXPERTS, D, EXPERT_FF), D ** -0.5),
        'w_up': nrm((L, N_EXPERTS, D, EXPERT_FF), D ** -0.5),
        'w_down': nrm((L, N_EXPERTS, EXPERT_FF, D), DEEPNORM_BETA * EXPERT_FF ** -0.5),
        'ln2_w': 1.0 + nrm((L, D), 0.02),
        'ln2_b': nrm((L, D), 0.02),
    }


def reference(x, c, ctx, c_ctx, w_mod, b_mod, w_in, q_norm, k_norm, attn_out_norm,
              ssd_conv_w, ssd_conv_b, ssd_dt_bias, ssd_a_log, ssd_d, ssd_norm,
              rg_conv_w, rg_conv_b, rg_wa, rg_ba, rg_wx, rg_bx, rg_lambda, rg_out_norm,
              w_out, ln1_w, ln1_b, w_router, w_gate, w_up, w_down, ln2_w, ln2_b):
    rope = axial_rope_tables(x.shape[1])
    for l in range(DEPTH):
        lp = dict(w_in=w_in[l], q_norm=q_norm[l], k_norm=k_norm[l], attn_out_norm=attn_out_norm[l],
                  ssd_conv_w=ssd_conv_w[l], ssd_conv_b=ssd_conv_b[l], ssd_dt_bias=ssd_dt_bias[l],
                  ssd_a_log=ssd_a_log[l], ssd_d=ssd_d[l], ssd_norm=ssd_norm[l],
                  rg_conv_w=rg_conv_w[l], rg_conv_b=rg_conv_b[l], rg_wa=rg_wa[l], rg_ba=rg_ba[l],
                  rg_wx=rg_wx[l], rg_bx=rg_bx[l], rg_lambda=rg_lambda[l], rg_out_norm=rg_out_norm[l],
                  w_out=w_out[l], ln1_w=ln1_w[l], ln1_b=ln1_b[l], w_router=w_router[l],
                  w_gate=w_gate[l], w_up=w_up[l], w_down=w_down[l], ln2_w=ln2_w[l], ln2_b=ln2_b[l])
        mod_x = jax.nn.silu(c) @ w_mod[l] + b_mod[l]
        mod_c = jax.nn.silu(c_ctx) @ w_mod[l] + b_mod[l]
        x, ctx = hybrid_layer(x, ctx, mod_x, mod_c, rope, lp, l == DEPTH - 1)
    return x
```

```python
from concourse.bass_utils import run_bass_kernel_spmd
import numpy as np
import concourse.bass as bass
import concourse.mybir as mybir
from contextlib import ExitStack

F32 = mybir.dt.float32
BF16 = mybir.dt.bfloat16
I32 = mybir.dt.int32
U32 = mybir.dt.uint32
AF = mybir.ActivationFunctionType
ALU = mybir.AluOpType
AX = mybir.AxisListType


class Res:
    __slots__ = ("name", "w", "r")

    def __init__(self, name=""):
        self.name = name
        self.w = []
        self.r = []


class FW:
    ENG = ("pe", "act", "dve", "pool", "sp")
    EPOCH = 30000
    NDMA = 12

    def __init__(self, nc, es):
        self.nc = nc
        self.es = es
        self.q = {e: [] for e in self.ENG}
        self.sems = {}
        self.cnt = {}
        self.cur = {}
        self.known = {e: {} for e in self.ENG}
        self.epoch = {e: 0 for e in self.ENG}
        for e in self.ENG:
            self._new_eng_sem(e)
        self.dma_pool = {}
        self.dma_rr = {}
        self.out_events = []
        self.n_inst = 0
        self.uid = 0
        self.tes = es

    def _sem(self, key):
        if key not in self.sems:
            self.sems[key] = self.es.enter_context(self.nc.semaphore("s_" + key))
            self.cnt[key] = 0
        return self.sems[key]

    def _new_eng_sem(self, e):
        key = f"{e}{self.epoch[e]}"
        self.epoch[e] += 1
        self._sem(key)
        self.cur[e] = key

    def sb(self, name, shape, dt):
        self.uid += 1
        return self.tes.enter_context(self.nc.sbuf_tensor(f"{name}_{self.uid}", list(shape), dt))

    def ps(self, name, shape, dt=F32):
        self.uid += 1
        return self.tes.enter_context(self.nc.psum_tensor(f"{name}_{self.uid}", list(shape), dt))

    def stage(self):
        fw = self

        class _S:
            def __enter__(s2):
                fw.barrier()
                s2.old = fw.tes
                s2.st = ExitStack()
                fw.tes = s2.st
                return s2

            def __exit__(s2, *a):
                fw.barrier()
                s2.st.close()
                fw.tes = s2.old
                return False
        return _S()

    def barrier(self):
        targets = [(k, c) for k, c in self.cnt.items() if c > 0]
        for eng in self.ENG:
            kn = self.known[eng]
            wl = []
            for k, c in targets:
                if kn.get(k, 0) < c:
                    kn[k] = c
                    wl.append((self.sems[k], c))
            if wl:
                def emit(e, wl=wl):
                    for s, v in wl:
                        e.wait_ge(s, v)
                self.q[eng].append(emit)

    def mm(self, out, lhsT, rhs, start, stop, reads=(), pwrites=()):
        return self.op("pe", lambda e: e.matmul(out, lhsT=lhsT, rhs=rhs, start=start, stop=stop), reads=reads, pwrites=pwrites)

    def tr(self, out, in_, ident, reads=(), pwrites=()):
        return self.op("pe", lambda e: e.transpose(out, in_, ident), reads=reads, pwrites=pwrites)

    def act(self, out, in_, func, reads=(), writes=(), pwrites=(), **kw):
        return self.op("act", lambda e: e.activation(out=out, in_=in_, func=func, **kw), reads=reads, writes=writes, pwrites=pwrites)

    def copy(self, eng, out, in_, reads=(), writes=(), pwrites=()):
        if eng == "act":
            return self.op("act", lambda e: e.activation(out=out, in_=in_, func=AF.Copy), reads=reads, writes=writes, pwrites=pwrites)
        return self.op(eng, lambda e: e.tensor_copy(out=out, in_=in_), reads=reads, writes=writes, pwrites=pwrites)

    def tt(self, eng, out, in0, in1, op, reads=(), writes=(), pwrites=()):
        return self.op(eng, lambda e: e.tensor_tensor(out=out, in0=in0, in1=in1, op=op), reads=reads, writes=writes, pwrites=pwrites)

    def ts(self, eng, out, in0, s1, s2, op0, op1=None, reads=(), writes=(), pwrites=()):
        if op1 is None:
            return self.op(eng, lambda e: e.tensor_scalar(out=out, in0=in0, scalar1=s1, scalar2=None, op0=op0), reads=reads, writes=writes, pwrites=pwrites)
        return self.op(eng, lambda e: e.tensor_scalar(out=out, in0=in0, scalar1=s1, scalar2=s2, op0=op0, op1=op1), reads=reads, writes=writes, pwrites=pwrites)

    def stt(self, eng, out, in0, scalar, in1, op0, op1, reads=(), writes=(), pwrites=()):
        return self.op(eng, lambda e: e.scalar_tensor_tensor(out=out, in0=in0, scalar=scalar, in1=in1, op0=op0, op1=op1), reads=reads, writes=writes, pwrites=pwrites)

    def memset(self, eng, ap, val, reads=(), writes=(), pwrites=()):
        return self.op(eng, lambda e: e.memset(ap, val), reads=reads, writes=writes, pwrites=pwrites)

    def _need(self, eng, reads, writes, pwrites=(), cls=None):
        need = {}
        if cls is None:
            cls = eng

        def add(ev, skip_same_pe=False):
            k, v, src = ev
            if skip_same_pe and src == "pe" and eng == "pe":
                return
            if need.get(k, 0) < v:
                need[k] = v
        for r in reads:
            for ev in r.w:
                add(ev)
        for w in writes:
            for ev in w.w:
                add(ev, skip_same_pe=True)
            for ev in w.r:
                add(ev)
        for w in pwrites:
            for ev in w.r:
                add(ev)
            for ev in w.w:
                if ev[2] != cls:
                    add(ev)
        out = []
        kn = self.known[eng]
        for k, v in need.items():
            if kn.get(k, 0) < v:
                kn[k] = v
                out.append((k, v))
        return out

    def _record(self, ev, reads, writes, pwrites=()):
        for r in reads:
            r.r = [e for e in r.r if e[0] != ev[0]] + [ev]
        for w in writes:
            w.w = [ev]
            w.r = []
        for w in pwrites:
            w.w = [e for e in w.w if e[0] != ev[0]] + [ev]

    def op(self, eng, fn, reads=(), writes=(), pwrites=()):
        reads = [r for r in reads if r is not None]
        writes = [w for w in writes if w is not None]
        pwrites = [w for w in pwrites if w is not None]
        waits = self._need(eng, reads, writes, pwrites)
        key = self.cur[eng]
        self.cnt[key] += 1
        val = self.cnt[key]
        sem = self.sems[key]
        wl = [(self.sems[k], v) for k, v in waits]

        def emit(e, fn=fn, wl=wl, sem=sem):
            for s, v in wl:
                e.wait_ge(s, v)
            fn(e).then_inc(sem, 1)
        self.q[eng].append(emit)
        self.n_inst += 1
        ev = (key, val, eng)
        self._record(ev, reads, writes, pwrites)
        if val >= self.EPOCH:
            self._new_eng_sem(eng)
        return ev

    def dma(self, queue, out, in_, reads=(), writes=(), pwrites=(), is_output=False, **kw):
        reads = [r for r in reads if r is not None]
        writes = [w for w in writes if w is not None]
        pwrites = [w for w in pwrites if w is not None]
        pool = self.dma_pool.setdefault(queue, [f"d{queue}{i}" for i in range(self.NDMA)])
        i = self.dma_rr.get(queue, 0)
        self.dma_rr[queue] = (i + 1) % len(pool)
        key = pool[i]
        sem = self._sem(key)
        if self.cnt[key] > 30000:
            nk = key + "n"
            pool[i] = nk
            prev_key, prev_val = key, self.cnt[key]
            key = nk
            sem = self._sem(key)
            extra = [(prev_key, prev_val)]
        else:
            extra = [(key, self.cnt[key])] if self.cnt[key] > 0 else []
        waits = self._need(queue, reads, writes, pwrites, cls='dma')
        kn = self.known[queue]
        for k, v in extra:
            if kn.get(k, 0) < v:
                kn[k] = v
                waits.append((k, v))
        self.cnt[key] += 16
        val = self.cnt[key]
        wl = [(self.sems[k], v) for k, v in waits]

        def emit(e, out=out, in_=in_, wl=wl, sem=sem, kw=kw):
            for s, v in wl:
                e.wait_ge(s, v)
            e.dma_start(out=out, in_=in_, **kw).then_inc(sem, 16)
        self.q[queue].append(emit)
        self.n_inst += 1
        ev = (key, val, "dma")
        self._record(ev, reads, writes, pwrites)
        if is_output:
            self.out_events.append(ev)
        return ev

    def dma_custom(self, queue, fn, reads=(), writes=(), pwrites=(), is_output=False):
        reads = [r for r in reads if r is not None]
        writes = [w for w in writes if w is not None]
        pwrites = [w for w in pwrites if w is not None]
        pool = self.dma_pool.setdefault(queue, [f"d{queue}{i}" for i in range(self.NDMA)])
        i = self.dma_rr.get(queue, 0)
        self.dma_rr[queue] = (i + 1) % len(pool)
        key = pool[i]
        sem = self._sem(key)
        extra = [(key, self.cnt[key])] if self.cnt[key] > 0 else []
        waits = self._need(queue, reads, writes, pwrites, cls='dma')
        kn = self.known[queue]
        for k, v in extra:
            if kn.get(k, 0) < v:
                kn[k] = v
                waits.append((k, v))
        self.cnt[key] += 16
        val = self.cnt[key]
        wl = [(self.sems[k], v) for k, v in waits]

        def emit(e, fn=fn, wl=wl, sem=sem):
            for s, v in wl:
                e.wait_ge(s, v)
            fn(e).then_inc(sem, 16)
        self.q[queue].append(emit)
        self.n_inst += 1
        ev = (key, val, "dma")
        self._record(ev, reads, writes, pwrites)
        if is_output:
            self.out_events.append(ev)
        return ev

    def finish(self):
        final_waits = [(self.sems[k], v) for (k, v, _) in self.out_events]
        q = self.q
        with self.nc.Block() as block:
            @block.tensor
            def _(e):
                for f in q["pe"]:
                    f(e)

            @block.scalar
            def _(e):
                for f in q["act"]:
                    f(e)

            @block.vector
            def _(e):
                for f in q["dve"]:
                    f(e)

            @block.gpsimd
            def _(e):
                for f in q["pool"]:
                    f(e)

            @block.sync
            def _(e):
                for f in q["sp"]:
                    f(e)
                for s, v in final_waits:
                    e.wait_ge(s, v)


D = 1024
NT = 4352
NTT = 34
BLOCKS = [(0, 256)] + [(256 + i * 512, 512) for i in range(8)]
EPS = 1e-6
ALPHA = 4 ** 0.25
C_ID, C_U, C_UT, C_NMF, C_NMB, C_ONES, C_BONES, C_RT = [i * 128 for i in range(8)]
PP = {}
_o = 0
for _n, _w in [("qw", 1), ("kw", 1), ("aon", 8), ("scw", 16), ("scb", 4), ("sd", 2), ("sn", 2),
               ("rcw", 8), ("rcb", 2), ("rba", 4), ("rbx", 4), ("rlam", 4), ("rn", 2)]:
    PP[_n] = (_o, _w)
    _o += _w
NPP = _o


def host_consts():
    c = np.zeros((128, 1024), np.float32)
    k = np.arange(128)[:, None]
    m = np.arange(128)[None, :]
    c[:, C_ID:C_ID + 128] = np.eye(128)
    c[:, C_U:C_U + 128] = (k <= m)
    c[:, C_UT:C_UT + 128] = (k >= m)
    c[:, C_NMF:C_NMF + 128] = np.where(k <= m, 0.0, -1e4)
    c[:, C_NMB:C_NMB + 128] = np.where(k >= m, 0.0, -1e4)
    c[:, C_ONES:C_ONES + 128] = 1.0
    c[:, C_BONES:C_BONES + 128] = ((k // 64) == (m // 64))
    rt = np.zeros((128, 128), np.float32)
    for base in (0, 64):
        for part in (0, 32):
            for d in range(16):
                mm_ = base + part + d
                rt[mm_ + 16, mm_] = -1.0
                rt[mm_, mm_ + 16] = 1.0
    c[:, C_RT:C_RT + 128] = rt
    return c


def host_rope():
    n = 4096
    t = np.arange(n)
    row = (t // 64).astype(np.float32)
    col = (t % 64).astype(np.float32)
    inv = (10000.0 ** (-np.arange(0, 32, 2, dtype=np.float32) / 32)).astype(np.float32)
    ang_r = row[:, None] * inv[None, :]
    ang_c = col[:, None] * inv[None, :]
    out = np.zeros((128, 2, n), np.float32)
    for p in range(128):
        d = p % 64
        a = ang_r if d < 32 else ang_c
        f = d % 16
        out[p, 0] = np.cos(a[:, f])
        out[p, 1] = np.sin(a[:, f])
    return out.reshape(128, 2 * n)


def host_pp(inp, l):
    pp = np.zeros((128, NPP), np.float32)

    def put(name, arr):
        o, w = PP[name]
        pp[:arr.shape[0], o:o + w] = arr.reshape(arr.shape[0], w)
    put("qw", np.tile(inp["q_norm"][l], 2)[:, None])
    put("kw", np.tile(inp["k_norm"][l], 2)[:, None])
    put("aon", inp["attn_out_norm"][l].reshape(8, 64).T)
    put("scw", inp["ssd_conv_w"][l].reshape(4, 4, 128).transpose(2, 1, 0))
    put("scb", inp["ssd_conv_b"][l].reshape(4, 128).T)
    put("sd", np.repeat(inp["ssd_d"][l], 64).reshape(2, 128).T)
    put("sn", inp["ssd_norm"][l].reshape(2, 128).T)
    put("rcw", inp["rg_conv_w"][l].reshape(4, 2, 128).transpose(2, 1, 0))
    put("rcb", inp["rg_conv_b"][l].reshape(2, 128).T)
    put("rba", inp["rg_ba"][l].reshape(2, 2, 128).transpose(2, 0, 1))
    put("rbx", inp["rg_bx"][l].reshape(2, 2, 128).transpose(2, 0, 1))
    put("rlam", inp["rg_lambda"][l].reshape(2, 2, 128).transpose(2, 0, 1))
    put("rn", inp["rg_out_norm"][l].reshape(2, 128).T)
    return pp


class DT:
    def __init__(self, nc, name, shape, dt, kind="Internal"):
        self.h = nc.dram_tensor(name, list(shape), dt, kind=kind)
        self.ap = self.h.ap()
        self.R = Res(name)
        self.name = name


IN_SHAPES = {
    "x": ([4096, 1024], F32), "ctx": ([256, 1024], F32), "cc": ([128, 16], F32),
    "b_mod": ([2, 6144], F32),
    "ssd_dt_bias": ([2, 8], F32), "ssd_a_log": ([2, 8], F32),
    "rg_wa": ([2, 2, 4, 64, 64], F32), "rg_wx": ([2, 2, 4, 64, 64], F32),
    "w_out": ([2, 1024, 1024], F32), "ln1_w": ([2, 1024], F32), "ln1_b": ([2, 1024], F32),
    "w_router": ([2, 1024, 16], F32), "ln2_w": ([2, 1024], F32), "ln2_b": ([2, 1024], F32),
    "pp": ([2, 128, NPP], F32), "consts": ([128, 1024], F32), "rope": ([128, 8192], F32),
}
for _l in range(2):
    IN_SHAPES[f"w_in{_l}"] = ([1024, 2056], F32)
    for _h in range(2):
        IN_SHAPES[f"w_mod{_l}_{_h}"] = ([512, 6144], F32)
    for _e in range(16):
        IN_SHAPES[f"wg{_l}_{_e}"] = ([1024, 2048], F32)
        IN_SHAPES[f"wu{_l}_{_e}"] = ([1024, 2048], F32)
        IN_SHAPES[f"wd{_l}_{_e}"] = ([2048, 1024], F32)
SCRATCH = {
    "modv": ([2, 2, 6144], F32), "projT": ([2056, NT], F32), "vtok": ([NT, 128], F32), "dttok": ([NT, 8], F32),
    "mergedT": ([1024, NT], BF16), "x1": ([4096, 1024], F32), "ctx1": ([256, 1024], F32),
    "xa": ([4096, 1024], F32), "ctxa": ([256, 1024], F32),
    "h2x": ([4096, 1024], BF16), "h2c": ([256, 1024], BF16), "accx": ([4096, 1024], F32), "accc": ([256, 1024], F32),
}


class G:
    gathered = False

    def moe_w(self, kind, l, e):
        t = getattr(self, {"gate": "wg", "up": "wu", "down": "wd"}[kind] + f"{l}_{e}")
        return t.ap, t.R


def make_G(nc, ext_in=(), ext_out=(), only=None):
    g = G()
    for n, (sh, dt) in IN_SHAPES.items():
        if only is not None and n not in only:
            continue
        setattr(g, n, DT(nc, n, sh, dt, kind="ExternalInput"))
    for n, (sh, dt) in SCRATCH.items():
        kind = "ExternalInput" if n in ext_in else ("ExternalOutput" if n in ext_out else "Internal")
        if only is not None and kind == "Internal" and n not in only:
            continue
        setattr(g, n, DT(nc, n, sh, dt, kind=kind))
    return g


def S0_mod(fw, g):
    with fw.stage():
        cc = fw.sb("cc", [128, 16], F32); Rcc = Res()
        sc = fw.sb("sc", [128, 16], F32); Rsc = Res()
        fw.dma("sp", cc[:], g.cc.ap, reads=[g.cc.R], writes=[Rcc])
        fw.act(sc[:], cc[:], AF.Silu, reads=[Rcc], writes=[Rsc])
        wm = [fw.sb(f"wm{i}", [128, 8, 512], F32) for i in range(2)]; Rwm = [Res(), Res()]
        bm = fw.sb("bm", [2, 6144], F32); Rbm = Res()
        mo = fw.sb("mo", [2, 6144], F32); Rmo = Res()
        ps = [fw.ps(f"mps{i}", [2, 512]) for i in range(2)]; Rps = [Res(), Res()]
        for l in range(2):
            for r in range(2):
                fw.dma("act", bm[r:r + 1, :], g.b_mod.ap[l:l + 1, :], reads=[g.b_mod.R], pwrites=[Rbm])
            wvs = [getattr(g, f"w_mod{l}_{h}") for h in range(2)]
            for n in range(12):
                b = n % 2
                for h in range(2):
                    fw.dma("sp", wm[b][:, h * 4:(h + 1) * 4, :], wvs[h].ap.rearrange("(j p) n -> p j n", p=128)[:, :, n * 512:(n + 1) * 512],
                           reads=[wvs[h].R], writes=[Rwm[b]] if h == 0 else [], pwrites=[] if h == 0 else [Rwm[b]])
                for j in range(8):
                    fw.mm(ps[b][:], sc[:, 2 * j:2 * j + 2], wm[b][:, j, :], j == 0, j == 7, reads=[Rsc, Rwm[b]], pwrites=[Rps[b]])
                fw.tt("dve", mo[:, n * 512:(n + 1) * 512], ps[b][:], bm[:, n * 512:(n + 1) * 512], ALU.add,
                      reads=[Rps[b], Rbm], pwrites=[Rmo])
            fw.dma("sp", g.modv.ap[l], mo[:], reads=[Rmo], pwrites=[g.modv.R])


def load_modcols(fw, g, l, which, dst, Rdst, eng="sp"):
    for r in range(2):
        src = g.modv.ap[l, r, which * 1024:(which + 1) * 1024].rearrange("(j p) -> p j", p=128)
        fw.dma(eng, dst[:, r, :], src, reads=[g.modv.R], pwrites=[Rdst], allow_slow_non_contiguous=True)


FCHUNKS = [(c * 128, 128) for c in range(12)] + [(1536, 8), (1544, 128), (1672, 128), (1800, 128), (1928, 128)]


def S1_inproj(fw, g, l, xsrc, csrc):
    with fw.stage():
        cons = fw.sb("cons", [128, 128], F32); Rcons = Res()
        fw.dma("sp", cons[:], g.consts.ap[:, C_ID:C_ID + 128], reads=[g.consts.R], writes=[Rcons])
        msc = fw.sb("msc", [128, 2, 8], F32); msh = fw.sb("msh", [128, 2, 8], F32); Rm = Res()
        load_modcols(fw, g, l, 0, msh, Rm)
        load_modcols(fw, g, l, 1, msc, Rm)
        fw.ts("dve", msc[:], msc[:], 1.0, None, ALU.add, reads=[Rm], pwrites=[Rm])
        win = fw.sb("win", [128, 8, 2056], BF16); Rwin = Res()
        for j in range(8):
            wi = getattr(g, f"w_in{l}")
            fw.dma("pool", win[:, j, :], wi.ap[j * 128:(j + 1) * 128, :], reads=[wi.R], pwrites=[Rwin])
        hxT = fw.sb("hxT", [128, 8, NT], BF16); RhxT = [Res() for _ in range(NTT)]
        xts = [fw.sb(f"xt{i}", [128, 1024], F32) for i in range(2)]; Rxt = [Res(), Res()]
        tps = [fw.ps(f"tp{i}", [128, 512]) for i in range(4)]; Rtp = [Res() for _ in range(4)]
        for tt in range(NTT):
            if tt < 2:
                src, sR, s = csrc.ap[tt * 128:(tt + 1) * 128, :], csrc.R, 1
            else:
                src, sR, s = xsrc.ap[(tt - 2) * 128:(tt - 1) * 128, :], xsrc.R, 0
            xt = xts[tt % 2]
            fw.dma("sp", xt[:], src, reads=[sR], writes=[Rxt[tt % 2]])
            for half in range(2):
                pi = (tt % 2) * 2 + half
                for jj in range(4):
                    j = half * 4 + jj
                    fw.tr(tps[pi][:, jj * 128:(jj + 1) * 128], xt[:, j * 128:(j + 1) * 128], cons[:],
                          reads=[Rxt[tt % 2], Rcons], pwrites=[Rtp[pi]])
                for jj in range(4):
                    j = half * 4 + jj
                    fw.act(hxT[:, j, tt * 128:(tt + 1) * 128], tps[pi][:, jj * 128:(jj + 1) * 128], AF.Identity,
                           bias=msh[:, s, j:j + 1], scale=msc[:, s, j:j + 1], reads=[Rtp[pi], Rm], pwrites=[RhxT[tt]])
        pps = [fw.ps(f"pp{i}", [128, 512]) for i in range(3)]; Rpp = [Res() for _ in range(3)]
        stg = [fw.sb(f"stg{i}", [128, 512], F32) for i in range(3)]; Rst = [Res() for _ in range(3)]
        k = 0
        for (t0, n) in BLOCKS:
            Rh = RhxT[t0 // 128:(t0 + n) // 128]
            for (c0, w) in FCHUNKS:
                b = k % 3
                for j in range(8):
                    fw.mm(pps[b][:w, :n], win[:, j, c0:c0 + w], hxT[:, j, t0:t0 + n], j == 0, j == 7,
                          reads=[Rwin] + Rh, pwrites=[Rpp[b]])
                fw.copy("act" if k % 2 == 0 else "dve", stg[b][:w, :n], pps[b][:w, :n], reads=[Rpp[b]], writes=[Rst[b]])
                fw.dma("sp" if k % 2 == 0 else "act", g.projT.ap[c0:c0 + w, t0:t0 + n], stg[b][:w, :n],
                       reads=[Rst[b]], pwrites=[g.projT.R])
                k += 1
        pv = fw.ps("pv", [128, 136]); Rpv = Res()
        vst = [fw.sb(f"vst{i}", [128, 136], F32) for i in range(2)]; Rvst = [Res(), Res()]
        for tt in range(NTT):
            sl = slice(tt * 128, (tt + 1) * 128)
            for j in range(8):
                fw.mm(pv[:, 0:128], hxT[:, j, sl], win[:, j, 640:768], j == 0, j == 7, reads=[Rwin, RhxT[tt]], pwrites=[Rpv])
            for j in range(8):
                fw.mm(pv[:, 128:136], hxT[:, j, sl], win[:, j, 1536:1544], j == 0, j == 7, reads=[Rwin, RhxT[tt]], pwrites=[Rpv])
            b = tt % 2
            fw.copy("dve", vst[b][:], pv[:], reads=[Rpv], writes=[Rvst[b]])
            fw.dma("sp", g.vtok.ap[sl, :], vst[b][:, 0:128], reads=[Rvst[b]], pwrites=[g.vtok.R])
            fw.dma("act", g.dttok.ap[sl, :], vst[b][:, 128:136], reads=[Rvst[b]], pwrites=[g.dttok.R])


def load_common(fw, g, l):
    cons = fw.sb("consA", [128, 1024], F32); Rc = Res()
    fw.dma("sp", cons[:], g.consts.ap, reads=[g.consts.R], writes=[Rc])
    pp = fw.sb("ppA", [128, NPP], F32); Rp = Res()
    fw.dma("act", pp[:], g.pp.ap[l], reads=[g.pp.R], writes=[Rp])
    return cons, Rc, pp, Rp


def ppc(pp, name, i=0, w=1):
    o, _ = PP[name]
    return pp[:, o + i:o + i + w]


def conv_seq(fw, g, row0, pad, Rpad, dst, Rdst, wcol, bcol, Rp):
    segs = [(0, 256, 0), (256, 4096, 260)]
    for (t0, L, po) in segs:
        fw.memset("pool", pad[:, po:po + 1], 0.0, pwrites=[Rpad])
        fw.memset("pool", pad[:, po + 1 + L:po + 3 + L], 0.0, pwrites=[Rpad])
        fw.dma("sp", pad[:, po + 1:po + 1 + L], g.projT.ap[row0:row0 + 128, t0:t0 + L], reads=[g.projT.R], pwrites=[Rpad])
    for (t0, L, po) in segs:
        fw.ts("dve", dst[:, t0:t0 + L], pad[:, po:po + L], wcol(0), bcol, ALU.mult, ALU.add, reads=[Rpad, Rp], pwrites=[Rdst])
        for j in range(1, 4):
            fw.stt("dve", dst[:, t0:t0 + L], pad[:, po + j:po + j + L], wcol(j), dst[:, t0:t0 + L], ALU.mult, ALU.add,
                   reads=[Rpad, Rp, Rdst], pwrites=[Rdst])


def rms_out(fw, g, src, Rsrc, nch, wcol, Rp, ones, Rc, row0, nfeat):
    sq = [fw.sb(f"rsq{i}", [128, nch, 512], F32) for i in range(2)]; Rsq = [Res(), Res()]
    rs = [fw.sb(f"rrs{i}", [128, 512], F32) for i in range(2)]; Rrs = [Res(), Res()]
    ob = [fw.sb(f"rob{i}", [128, nch, 512], BF16) for i in range(2)]; Rob = [Res(), Res()]
    pss = [fw.ps(f"rps{i}", [128, 512]) for i in range(2)]; Rps = [Res(), Res()]
    for bi, (t0, n) in enumerate(BLOCKS):
        b = bi % 2
        fw.act(sq[b][:, :, :n], src[:, :, t0:t0 + n], AF.Square, reads=[Rsrc], writes=[Rsq[b]])
        for c in range(nch):
            fw.mm(pss[b][:, :n], ones, sq[b][:, c, :n], c == 0, c == nch - 1, reads=[Rsq[b], Rc], pwrites=[Rps[b]])
        fw.act(rs[b][:, :n], pss[b][:, :n], AF.Ln, bias=EPS, scale=1.0 / nfeat, reads=[Rps[b]], writes=[Rrs[b]])
        fw.act(rs[b][:, :n], rs[b][:, :n], AF.Exp, scale=-0.5, reads=[Rrs[b]], writes=[Rrs[b]])
        for c in range(nch):
            fw.stt("dve", ob[b][:, c, :n], src[:, c, t0:t0 + n], wcol(c), rs[b][:, :n], ALU.mult, ALU.mult,
                   reads=[Rsrc, Rrs[b], Rp], pwrites=[Rob[b]])
            fw.dma("sp", g.mergedT.ap[row0 + c * 128:row0 + (c + 1) * 128, t0:t0 + n], ob[b][:, c, :n],
                   reads=[Rob[b]], pwrites=[g.mergedT.R])


def S2_rg(fw, g, l):
    with fw.stage():
        cons, Rc, pp, Rp = load_common(fw, g, l)
        ones = cons[:, C_ONES:C_ONES + 128]
        wg = fw.sb("rgw", [128, 8, 128], F32); Rwg = Res()
        fw.memset("pool", wg[:], 0.0, writes=[Rwg])
        for gate, W in enumerate((g.rg_wa, g.rg_wx)):
            for d in range(2):
                for cc in range(2):
                    for i in range(2):
                        fw.dma("sp", wg[i * 64:(i + 1) * 64, gate * 4 + d * 2 + cc, i * 64:(i + 1) * 64],
                               W.ap[l, d, 2 * cc + i], reads=[W.R], pwrites=[Rwg])
        spl = fw.sb("spl", [128, 4], F32); m8 = fw.sb("m8", [128, 4], F32); m16 = fw.sb("m16", [128, 4], F32); Rs = Res()
        fw.act(spl[:], ppc(pp, "rlam", 0, 4), AF.Exp, scale=-1.0, reads=[Rp], writes=[Rs])
        fw.act(spl[:], spl[:], AF.Ln, bias=1.0, scale=1.0, reads=[Rs], writes=[Rs])
        fw.ts("dve", m8[:], spl[:], -8.0, None, ALU.mult, reads=[Rs], pwrites=[Rs])
        fw.ts("dve", m16[:], spl[:], -16.0000001, None, ALU.mult, reads=[Rs], pwrites=[Rs])
        A = fw.sb("rgA", [128, 4360], F32); RA = Res()
        u = fw.sb("rgu", [128, NT], F32); Ru = Res()
        rt = fw.sb("rgr", [128, NT], F32); Rrt = Res()
        it = fw.sb("rgi", [128, NT], F32); Rit = Res()
        tmp = fw.sb("rgt", [128, NT], F32); Rtmp = Res()
        hs = fw.sb("rgh", [128, NT], F32); Rhs = Res()
        yg = fw.sb("rgy", [128, 2, NT], F32); Ryg = Res()
        pr = [fw.ps(f"rgp{i}", [128, 512]) for i in range(4)]; Rpr = [Res() for _ in range(4)]
        for cc in range(2):
            conv_seq(fw, g, 1544 + cc * 128, A, RA, u, Ru, lambda j: ppc(pp, "rcw", cc * 4 + j), ppc(pp, "rcb", cc), Rp)
            for d in range(2):
                k = 0
                for (t0, n) in BLOCKS:
                    for gate, (dst, Rd, bn) in enumerate(((rt, Rrt, "rba"), (it, Rit, "rbx"))):
                        b = k % 4; k += 1
                        fw.mm(pr[b][:, :n], wg[:, gate * 4 + d * 2 + cc, :], u[:, t0:t0 + n], True, True,
                              reads=[Rwg, Ru], pwrites=[Rpr[b]])
                        fw.act(dst[:, t0:t0 + n], pr[b][:, :n], AF.Sigmoid, bias=ppc(pp, bn, d * 2 + cc), scale=1.0,
                               reads=[Rpr[b], Rp], pwrites=[Rd])
                sc8 = m8[:, d * 2 + cc:d * 2 + cc + 1]; sc16 = m16[:, d * 2 + cc:d * 2 + cc + 1]
                fw.act(tmp[:], rt[:], AF.Exp, scale=sc16, reads=[Rrt, Rs], writes=[Rtmp])
                fw.ts("dve", tmp[:], tmp[:], -1.0, 1.0, ALU.mult, ALU.add, reads=[Rtmp], writes=[Rtmp])
                fw.ts("dve", tmp[:], tmp[:], 0.0, None, ALU.max, reads=[Rtmp], writes=[Rtmp])
                fw.act(tmp[:], tmp[:], AF.Sqrt, reads=[Rtmp], writes=[Rtmp])
                fw.act(rt[:], rt[:], AF.Exp, scale=sc8, reads=[Rrt, Rs], writes=[Rrt])
                fw.tt("dve", it[:], it[:], tmp[:], ALU.mult, reads=[Rit, Rtmp], writes=[Rit])
                fw.tt("dve", it[:], it[:], u[:], ALU.mult, reads=[Rit, Ru], writes=[Rit])
                out = hs if d == 0 else tmp
                Ro = Rhs if d == 0 else Rtmp
                if d == 0:
                    fw.op("dve", lambda e, out=out: e.tensor_tensor_scan(out=out[:, 0:256], data0=rt[:, 0:256], data1=it[:, 0:256],
                          initial=0.0, op0=ALU.mult, op1=ALU.add), reads=[Rrt, Rit], writes=[Ro])
                    fw.op("dve", lambda e, out=out: e.tensor_tensor_scan(out=out[:, 256:NT], data0=rt[:, 256:NT], data1=it[:, 256:NT],
                          initial=out[:, 255:256], op0=ALU.mult, op1=ALU.add), reads=[Rrt, Rit, Ro], pwrites=[Ro])
                else:
                    fw.op("dve", lambda e, out=out: e.tensor_tensor_scan(out=out[:, 0:256][:, ::-1], data0=rt[:, 0:256][:, ::-1],
                          data1=it[:, 0:256][:, ::-1], initial=0.0, op0=ALU.mult, op1=ALU.add), reads=[Rrt, Rit], writes=[Ro])
                    fw.op("dve", lambda e, out=out: e.tensor_tensor_scan(out=out[:, 256:NT][:, ::-1], data0=rt[:, 256:NT][:, ::-1],
                          data1=it[:, 256:NT][:, ::-1], initial=out[:, 0:1], op0=ALU.mult, op1=ALU.add), reads=[Rrt, Rit, Ro], pwrites=[Ro])
                    fw.tt("dve", hs[:], hs[:], tmp[:], ALU.add, reads=[Rhs, Rtmp], writes=[Rhs])
            gt = A[:, 0:NT]
            fw.dma("sp", gt, g.projT.ap[1800 + cc * 128:1800 + (cc + 1) * 128, :], reads=[g.projT.R], writes=[RA])
            fw.act(tmp[:], gt, AF.Square, reads=[RA], writes=[Rtmp])
            fw.ts("dve", tmp[:], tmp[:], 0.044715, 1.0, ALU.mult, ALU.add, reads=[Rtmp], writes=[Rtmp])
            fw.tt("dve", tmp[:], tmp[:], gt, ALU.mult, reads=[Rtmp, RA], writes=[Rtmp])
            fw.act(tmp[:], tmp[:], AF.Sigmoid, scale=1.5957691216057308, reads=[Rtmp], writes=[Rtmp])
            fw.tt("dve", tmp[:], tmp[:], gt, ALU.mult, reads=[Rtmp, RA], writes=[Rtmp])
            fw.tt("dve", yg[:, cc, :], hs[:], tmp[:], ALU.mult, reads=[Rhs, Rtmp], pwrites=[Ryg])
        rms_out(fw, g, yg, Ryg, 2, lambda c: ppc(pp, "rn", c), Rp, ones, Rc, 768, 256)


DBG = {}


def S3_ssd(fw, g, l):
    with fw.stage():
        cons, Rc, pp, Rp = load_common(fw, g, l)
        ident = cons[:, C_ID:C_ID + 128]
        ones = cons[:, C_ONES:C_ONES + 128]
        A = fw.sb("sA", [128, 4360], F32); RA = Res()
        xa = fw.sb("sxa", [128, 4, NT], F32); Rxa = [Res() for _ in range(4)]
        for c in range(4):
            conv_seq(fw, g, 768 + c * 128, A, RA, xa[:, c, :], Rxa[c], lambda j: ppc(pp, "scw", c * 4 + j), ppc(pp, "scb", c), Rp)
            fw.act(xa[:, c, :], xa[:, c, :], AF.Silu, reads=[Rxa[c]], writes=[Rxa[c]])
        dtt = fw.sb("sdt", [128, NTT, 8], F32); Rdt = Res()
        da = fw.sb("sda", [128, NTT, 8], F32); Rda = Res()
        bb = fw.sb("sbb", [128, 2, 8], F32); Rbb = Res()
        for tt in range(NTT):
            fw.dma("sp" if tt % 2 else "act", dtt[:, tt, :], g.dttok.ap[tt * 128:(tt + 1) * 128, :], reads=[g.dttok.R], pwrites=[Rdt])
        fw.dma("act", bb[:, 0, :], g.ssd_dt_bias.ap[l].partition_broadcast(128), reads=[g.ssd_dt_bias.R], pwrites=[Rbb])
        fw.dma("act", bb[:, 1, :], g.ssd_a_log.ap[l].partition_broadcast(128), reads=[g.ssd_a_log.R], pwrites=[Rbb])
        fw.act(bb[:, 1, :], bb[:, 1, :], AF.Exp, reads=[Rbb], pwrites=[Rbb])
        fw.ts("dve", bb[:, 1, :], bb[:, 1, :], -1.0, None, ALU.mult, reads=[Rbb], pwrites=[Rbb])
        for tt in range(NTT):
            fw.tt("dve", dtt[:, tt, :], dtt[:, tt, :], bb[:, 0, :], ALU.add, reads=[Rdt, Rbb], pwrites=[Rdt])
        dtf = dtt[:].rearrange("p t e -> p (t e)")
        fw.act(dtf, dtf, AF.Exp, reads=[Rdt], writes=[Rdt])
        fw.act(dtf, dtf, AF.Ln, bias=1.0, scale=1.0, reads=[Rdt], writes=[Rdt])
        for tt in range(NTT):
            fw.tt("dve", da[:, tt, :], dtt[:, tt, :], bb[:, 1, :], ALU.mult, reads=[Rdt, Rbb], pwrites=[Rda])
        yT = fw.sb("syT", [128, 2, NT], F32); RyT = Res()
        SinP = fw.sb("sSin", [128, 2, 128], F32); RS = [Res(), Res()]
        fw.memset("pool", SinP[:], 0.0, writes=RS)
        xtok = fw.sb("sxtok", [128, 384], F32); Rxtok = Res()
        rhsU = fw.sb("srhsU", [128, 4, 128], F32); RrhsU = Res()
        cscol = fw.sb("scscol", [128, 4], F32); Rcscol = Res()
        Ecs = fw.sb("sEcs", [128, 4, 128], F32); REcs = Res()
        csb = fw.sb("scsb", [128, 4, 128], F32); Rcsb = Res()
        dm = fw.sb("sdm", [128, 4, 128], F32); Rdm = Res()
        dcol = fw.sb("sdcol", [128, 4], F32); Rdcol = Res()
        xdt = fw.sb("sxdt", [128, 4, 64], F32); Rxdt = Res()
        xdd = fw.sb("sxdd", [128, 4, 64], F32); Rxdd = Res()
        Ce = fw.sb("sCe", [128, 4, 128], F32); RCe = Res()
        Mt = fw.sb("sMt", [128, 4, 128], F32); RMt = Res()
        Bm = fw.sb("sBm", [128, 2, 128], F32); RBm = Res()
        ptk = fw.ps("sptk", [128, 512]); Rptk = Res()
        pcs = fw.ps("spcs", [128, 4, 128]); Rpcs = Res()
        pcc = fw.ps("spcc", [128, 512]); Rpcc = Res()
        Gsb = fw.sb("sGsb", [128, 2, 128], F32); RGsb = Res()
        ysb = fw.sb("sysb", [128, 4, 128], F32); Rysb = Res()
        Ssb = fw.sb("sSsb", [128, 2, 128], F32); RSsb = Res()
        pG = fw.ps("spG", [128, 4, 128]); RpG = Res()
        py = fw.ps("spy", [128, 4, 128]); Rpy = Res()
        pS = fw.ps("spS", [128, 4, 128]); RpS = Res()
        for d in range(2):
            order = list(range(NTT)) if d == 0 else [1, 0] + list(range(NTT - 1, 1, -1))
            Um = cons[:, C_U:C_U + 128] if d == 0 else cons[:, C_UT:C_UT + 128]
            nm = cons[:, C_NMF:C_NMF + 128] if d == 0 else cons[:, C_NMB:C_NMB + 128]
            ec = 127 if d == 0 else 0
            for tt in order[:DBG.get('units', 99)]:
                sl = slice(tt * 128, (tt + 1) * 128)
                for c in range(3):
                    fw.tr(ptk[:, c * 128:(c + 1) * 128], xa[:, c, sl], ident, reads=[Rxa[c], Rc], pwrites=[Rptk])
                fw.copy("act", xtok[:], ptk[:, 0:384], reads=[Rptk], writes=[Rxtok])
                if DBG.get('step', 99) < 2:
                    continue
                for h in range(4):
                    fw.ts("dve", rhsU[:, h, :], Um, da[:, tt, d * 4 + h:d * 4 + h + 1], None, ALU.mult, reads=[Rc, Rda], pwrites=[RrhsU])
                fw.mm(pcs[:].rearrange("p h l -> p (h l)"), ones, rhsU[:].rearrange("p h l -> p (h l)"), True, True,
                      reads=[Rc, RrhsU], pwrites=[Rpcs])
                fw.mm(pcc[:, 0:4], Um, da[:, tt, d * 4:d * 4 + 4], True, True, reads=[Rc, Rda], pwrites=[Rpcc])
                fw.copy("act", cscol[:], pcc[:, 0:4], reads=[Rpcc], writes=[Rcscol])
                if DBG.get('step', 99) < 3:
                    continue
                fw.copy("dve", csb[:].rearrange("p h l -> p (h l)"), pcs[:].rearrange("p h l -> p (h l)"), reads=[Rpcs], writes=[Rcsb])
                fw.act(Ecs[:].rearrange("p h l -> p (h l)"), csb[:].rearrange("p h l -> p (h l)"), AF.Exp, reads=[Rcsb], writes=[REcs])
                for h in range(4):
                    fw.stt("dve", dm[:, h, :], csb[:, h, :], cscol[:, h:h + 1], nm, ALU.subtract, ALU.min,
                           reads=[Rcsb, Rcscol, Rc], pwrites=[Rdm])
                fw.act(dm[:].rearrange("p h l -> p (h l)"), dm[:].rearrange("p h l -> p (h l)"), AF.Exp, reads=[Rdm], writes=[Rdm])
                fw.tt("dve", dcol[:], csb[:, :, ec], cscol[:], ALU.subtract, reads=[Rcsb, Rcscol], writes=[Rdcol])
                fw.act(dcol[:], dcol[:], AF.Exp, reads=[Rdcol], writes=[Rdcol])
                if DBG.get('step', 99) < 4:
                    continue
                for h in range(4):
                    fw.ts("dve", xdt[:, h, :], xtok[:, h * 64:(h + 1) * 64], dtt[:, tt, d * 4 + h:d * 4 + h + 1], None, ALU.mult,
                          reads=[Rxtok, Rdt], pwrites=[Rxdt])
                for h in range(4):
                    fw.ts("dve", xdd[:, h, :], xdt[:, h, :], dcol[:, h:h + 1], None, ALU.mult, reads=[Rxdt, Rdcol], pwrites=[Rxdd])
                if DBG.get('step', 99) < 5:
                    continue
                for h in range(4):
                    mcol = cons[:, C_BONES + 64 * (h // 2):C_BONES + 64 * (h // 2) + 1]
                    fw.stt("dve", Ce[:, h, :], xa[:, 3, sl], mcol, Ecs[:, h, :], ALU.mult, ALU.mult, reads=[Rxa[3], REcs, Rc], pwrites=[RCe])
                for gg in range(2):
                    mcol = cons[:, C_BONES + 64 * gg:C_BONES + 64 * gg + 1]
                    fw.ts("dve", Bm[:, gg, :], xa[:, 2, sl], mcol, None, ALU.mult, reads=[Rxa[2], Rc], pwrites=[RBm])
                for gg in range(2):
                    fw.mm(pG[:, gg, :], Bm[:, gg, :], xa[:, 3, sl], True, True, reads=[RBm, Rxa[3]], pwrites=[RpG])
                fw.copy("act", Gsb[:].rearrange("p a b -> p (a b)"), pG[:, 0:2, :].rearrange("p a b -> p (a b)"), reads=[RpG], writes=[RGsb])
                for h in range(4):
                    fw.tt("dve", Mt[:, h, :], Gsb[:, h // 2, :], dm[:, h, :], ALU.mult, reads=[RGsb, Rdm], pwrites=[RMt])
                if DBG.get('step', 99) < 6:
                    continue
                for h in range(4):
                    j = h // 2
                    ps_ = slice(j * 64, (j + 1) * 64)
                    fw.mm(py[:, h, :], xdt[:, 2 * j:2 * j + 2, :].rearrange("p a b -> p (a b)"), Mt[:, h, :], True, False,
                          reads=[Rxdt, RMt], pwrites=[Rpy])
                    fw.mm(py[:, h, :], SinP[:, d, :], Ce[:, h, :], False, True, reads=[RS[d], RCe], pwrites=[Rpy])
                if DBG.get('step', 99) < 7:
                    continue
                for h in range(4):
                    j, i = h // 2, h % 2
                    rows = slice(i * 64, (i + 1) * 64)
                    if h == 0:
                        fw.copy("act", ysb[:].rearrange("p a b -> p (a b)"), py[:].rearrange("p a b -> p (a b)"), reads=[Rpy], writes=[Rysb])
                    if d == 0:
                        fw.copy("dve", yT[rows, j, sl], ysb[rows, h, :], reads=[Rysb], pwrites=[RyT])
                    else:
                        fw.tt("dve", yT[rows, j, sl], ysb[rows, h, :], yT[rows, j, sl], ALU.add, reads=[Rysb, RyT], pwrites=[RyT])
                if DBG.get('step', 99) < 8:
                    continue
                for j in range(2):
                    fw.mm(pS[:, j, :], xtok[:, 256:384], xdd[:, 2 * j:2 * j + 2, :].rearrange("p a b -> p (a b)"), True, True,
                          reads=[Rxtok, Rxdd], pwrites=[RpS])
                fw.copy("act", Ssb[:].rearrange("p a b -> p (a b)"), pS[:, 0:2, :].rearrange("p a b -> p (a b)"), reads=[RpS], writes=[RSsb])
                for h in range(4):
                    j, i = h // 2, h % 2
                    rows = slice(j * 64, (j + 1) * 64)
                    cols = slice(i * 64, (i + 1) * 64)
                    fw.stt("dve", SinP[rows, d, cols], SinP[rows, d, cols], Ecs[rows, h, ec:ec + 1], Ssb[rows, j, cols], ALU.mult, ALU.add,
                           reads=[RS[d], REcs, RSsb], pwrites=[RS[d]])
        for j in range(2):
            fw.stt("dve", yT[:, j, :], xa[:, j, :], ppc(pp, "sd", j), yT[:, j, :], ALU.mult, ALU.add, reads=[Rxa[j], Rp, RyT], pwrites=[RyT])
            zt = A[:, 0:NT]
            fw.dma("sp", zt, g.projT.ap[1280 + j * 128:1280 + (j + 1) * 128, :], reads=[g.projT.R], writes=[RA])
            fw.act(zt, zt, AF.Silu, reads=[RA], writes=[RA])
            fw.tt("dve", yT[:, j, :], yT[:, j, :], zt, ALU.mult, reads=[RyT, RA], pwrites=[RyT])
        rms_out(fw, g, yT, RyT, 2, lambda c: ppc(pp, "sn", c), Rp, ones, Rc, 512, 256)


class LNCtx:
    def __init__(self, fw, g, l, wname, bname):
        self.w = fw.sb("lnw", [128, 1024], F32); self.b = fw.sb("lnb", [128, 1024], F32); self.R = Res()
        W = getattr(g, wname); B = getattr(g, bname)
        fw.dma("sp", self.w[:], W.ap[l].partition_broadcast(128), reads=[W.R], pwrites=[self.R])
        fw.dma("act", self.b[:], B.ap[l].partition_broadcast(128), reads=[B.R], pwrites=[self.R])
        self.sq = fw.sb("lnsq", [128, 1024], F32); self.Rsq = Res()
        self.st = fw.sb("lnst", [128, 8], F32); self.Rst = Res()
        self.o = [fw.sb(f"lno{i}", [128, 1024], F32) for i in range(2)]; self.Ro = [Res(), Res()]
        self.k = 0


def ln_tile(fw, L, y, Ry, dst_ap, dstR, is_output=False):
    st = L.st
    fw.op("dve", lambda e: e.tensor_reduce(out=st[:, 0:1], in_=y, axis=AX.X, op=ALU.add), reads=[Ry], pwrites=[L.Rst])
    fw.act(L.sq[:], y, AF.Square, reads=[Ry], writes=[L.Rsq])
    fw.op("dve", lambda e: e.tensor_reduce(out=st[:, 1:2], in_=L.sq[:], axis=AX.X, op=ALU.add), reads=[L.Rsq], pwrites=[L.Rst])
    fw.ts("dve", st[:, 2:3], st[:, 0:1], 1.0 / 1024, None, ALU.mult, reads=[L.Rst], pwrites=[L.Rst])
    fw.tt("dve", st[:, 3:4], st[:, 2:3], st[:, 2:3], ALU.mult, reads=[L.Rst], pwrites=[L.Rst])
    fw.stt("dve", st[:, 4:5], st[:, 1:2], 1.0 / 1024, st[:, 3:4], ALU.mult, ALU.subtract, reads=[L.Rst], pwrites=[L.Rst])
    fw.act(st[:, 5:6], st[:, 4:5], AF.Ln, bias=EPS, scale=1.0, reads=[L.Rst], pwrites=[L.Rst])
    fw.act(st[:, 5:6], st[:, 5:6], AF.Exp, scale=-0.5, reads=[L.Rst], pwrites=[L.Rst])
    fw.stt("dve", st[:, 6:7], st[:, 2:3], -1.0, st[:, 5:6], ALU.mult, ALU.mult, reads=[L.Rst], pwrites=[L.Rst])
    b = L.k % 2; L.k += 1
    o = L.o[b]
    fw.act(o[:], y, AF.Identity, bias=st[:, 6:7], scale=st[:, 5:6], reads=[Ry, L.Rst], writes=[L.Ro[b]])
    fw.tt("dve", o[:], o[:], L.w[:], ALU.mult, reads=[L.Ro[b], L.R], writes=[L.Ro[b]])
    fw.tt("dve", o[:], o[:], L.b[:], ALU.add, reads=[L.Ro[b], L.R], writes=[L.Ro[b]])
    fw.dma("sp", dst_ap, o[:], reads=[L.Ro[b]], pwrites=[dstR], is_output=is_output)


def S4_attn(fw, g, l, xsrc, csrc, xdst, cdst, do_ctx):
    with fw.stage():
        cons, Rc, pp, Rp = load_common(fw, g, l)
        ident = cons[:, C_ID:C_ID + 128]; ones = cons[:, C_ONES:C_ONES + 128]
        bones = cons[:, C_BONES:C_BONES + 128]; Rtm = cons[:, C_RT:C_RT + 128]
        g1b = fw.sb("g1b", [128, 2, 1024], F32); Rg1 = Res()
        for r in range(2):
            fw.dma("sp", g1b[:, r, :], g.modv.ap[l, r, 2048:3072].partition_broadcast(128), reads=[g.modv.R], pwrites=[Rg1])
        L = LNCtx(fw, g, l, "ln1_w", "ln1_b")
        woa = fw.sb("woa", [64, 8, 1024], BF16); wob = fw.sb("wob", [128, 4, 1024], BF16); Rwo = Res()
        for h in range(8):
            fw.dma("pool", woa[:, h, :], g.w_out.ap[l, h * 64:(h + 1) * 64, :], reads=[g.w_out.R], pwrites=[Rwo])
        for c in range(4):
            fw.dma("pool", wob[:, c, :], g.w_out.ap[l, 512 + c * 128:512 + (c + 1) * 128, :], reads=[g.w_out.R], pwrites=[Rwo])
        Vx = fw.sb("Vx", [128, NTT, 2, 128], BF16); RVx = Res()
        fw.memset("pool", Vx[:].rearrange("p t g d -> p (t g d)"), 1.0, writes=[RVx])
        for tt in range(NTT):
            fw.dma("pool", Vx[:, tt, :, 0:64], g.vtok.ap[tt * 128:(tt + 1) * 128, :].rearrange("p (g d) -> p g d", g=2),
                   reads=[g.vtok.R], pwrites=[RVx])
        kTm = fw.sb("kTm", [128, 2, NT], BF16); RkT = Res()
        raw = [fw.sb(f"raw{i}", [128, 512], F32) for i in range(2)]; Rraw = [Res(), Res()]
        sq = fw.sb("qsq", [128, 512], F32); Rsq = Res()
        rs = fw.sb("qrs", [128, 512], F32); Rrs = Res()
        kn = fw.sb("qkn", [128, 512], F32); Rkn = Res()
        t1 = fw.sb("qt1", [128, 512], F32); Rt1 = Res()
        t2 = fw.sb("qt2", [128, 512], F32); Rt2 = Res()
        rope = fw.sb("ropeb", [128, 2, 512], F32); Rrope = Res()
        pprep = [fw.ps(f"pprep{i}", [128, 512]) for i in range(2)]; Rpp_ = [Res(), Res()]
        cnt = [0]

        def qk_block(n, wcol, rope_t0, outs):
            b = cnt[0] % 2
            r_ = raw[b]; Rr = Rraw[b]
            fw.act(sq[:, :n], r_[:, :n], AF.Square, reads=[Rr], writes=[Rsq])
            fw.mm(pprep[0][:, :n], bones, sq[:, :n], True, True, reads=[Rc, Rsq], pwrites=[Rpp_[0]])
            fw.act(rs[:, :n], pprep[0][:, :n], AF.Ln, bias=EPS, scale=1.0 / 64, reads=[Rpp_[0]], writes=[Rrs])
            fw.act(rs[:, :n], rs[:, :n], AF.Exp, scale=-0.5, reads=[Rrs], writes=[Rrs])
            fw.stt("dve", kn[:, :n], r_[:, :n], wcol, rs[:, :n], ALU.mult, ALU.mult, reads=[Rr, Rp, Rrs], writes=[Rkn])
            src, Rsrc = kn, Rkn
            if rope_t0 is not None:
                fw.dma("sp", rope[:, 0, :n], g.rope.ap[:, rope_t0:rope_t0 + n], reads=[g.rope.R], pwrites=[Rrope])
                fw.dma("act", rope[:, 1, :n], g.rope.ap[:, 4096 + rope_t0:4096 + rope_t0 + n], reads=[g.rope.R], pwrites=[Rrope])
                fw.mm(pprep[1][:, :n], Rtm, kn[:, :n], True, True, reads=[Rc, Rkn], pwrites=[Rpp_[1]])
                fw.tt("dve", t2[:, :n], pprep[1][:, :n], rope[:, 1, :n], ALU.mult, reads=[Rpp_[1], Rrope], writes=[Rt2])
                fw.tt("dve", t1[:, :n], kn[:, :n], rope[:, 0, :n], ALU.mult, reads=[Rkn, Rrope], writes=[Rt1])
                fw.tt("dve", t1[:, :n], t1[:, :n], t2[:, :n], ALU.add, reads=[Rt1, Rt2], writes=[Rt1])
                src, Rsrc = t1, Rt1
            for (oap, mcol, Ro) in outs:
                if mcol is None:
                    fw.copy("dve", oap, src[:, :n], reads=[Rsrc], pwrites=[Ro])
                else:
                    fw.ts("dve", oap, src[:, :n], mcol, None, ALU.mult, reads=[Rsrc, Rc], pwrites=[Ro])
            cnt[0] += 1

        for (t0, n) in BLOCKS:
            b = cnt[0] % 2
            fw.dma("sp", raw[b][:, :n], g.projT.ap[512:640, t0:t0 + n], reads=[g.projT.R], writes=[Rraw[b]])
            qk_block(n, ppc(pp, "kw"), None if t0 < 256 else t0 - 256,
                     [(kTm[:, gg, t0:t0 + n], cons[:, C_BONES + 64 * gg:C_BONES + 64 * gg + 1], RkT) for gg in range(2)])
        qT = fw.sb("qT", [128, 4, 512], BF16); RqT = Res()
        pT = [fw.sb(f"pT{i}", [128, 512], BF16) for i in range(3)]; RpT = [Res() for _ in range(3)]
        osb = fw.sb("osb", [128, 512], F32); Rosb = Res()
        denb = fw.sb("denb", [64, 512], F32); Rdenb = Res()
        attnb = fw.sb("attnb", [64, 8, 512], F32); Rattnb = Res()
        sqb = fw.sb("sqb", [64, 8, 512], F32); Rsqb = Res()
        attn_n = fw.sb("attn_n", [64, 8, 512], BF16); Rattn_n = Res()
        sr = fw.sb("sr", [128, 4, 512], BF16); Rsr = Res()
        xt = [fw.sb(f"xt{i}", [128, 1024], F32) for i in range(2)]; Rxt = [Res(), Res()]
        ysb = [fw.sb(f"ysb{i}", [128, 1024], F32) for i in range(2)]; Rysb = [Res(), Res()]
        psc = [fw.ps(f"psc{i}", [128, 512]) for i in range(2)]; Rpsc = [Res(), Res()]
        po = [fw.ps(f"po{i}", [128, 512]) for i in range(2)]; Rpo = [Res(), Res()]
        pso = [fw.ps(f"pso{i}", [128, 512]) for i in range(2)]; Rpso = [Res(), Res()]
        blocks = [(256 + i * 512, 512, list(range(NTT)), 0) for i in range(8)]
        if do_ctx:
            blocks.append((0, 256, [0, 1], 1))
        kk = 0
        hk = 0
        tk = 0
        for (t0, n, ktiles, r) in blocks:
            for jp in range(4):
                b = cnt[0] % 2
                fw.dma("sp", raw[b][0:64, :n], g.projT.ap[jp * 64:(jp + 1) * 64, t0:t0 + n], reads=[g.projT.R], pwrites=[Rraw[b]])
                fw.dma("act", raw[b][64:128, :n], g.projT.ap[(jp + 4) * 64:(jp + 5) * 64, t0:t0 + n], reads=[g.projT.R], pwrites=[Rraw[b]])
                qk_block(n, ppc(pp, "qw"), None if r == 1 else t0 - 256, [(qT[:, jp, :n], None, RqT)])
            for c in range(4):
                fw.dma("act", sr[:, c, :n], g.mergedT.ap[512 + c * 128:512 + (c + 1) * 128, t0:t0 + n], reads=[g.mergedT.R], pwrites=[Rsr])
            its = []
            for jp in range(4):
                for half in range(2):
                    h = jp + 4 * half
                    pob = hk % 2; hk += 1
                    for ki, kc in enumerate(ktiles):
                        its.append((jp, half, h, pob, ki, kc, kk % 2, kk % 3))
                        kk += 1

            def emit_pv(it):
                jp, half, h, pob, ki, kc, sb_, pb = it
                fw.mm(po[pob][:, :n], Vx[:, kc, half, :], pT[pb][:, :n], ki == 0, ki == len(ktiles) - 1,
                      reads=[RVx, RpT[pb]], pwrites=[Rpo[pob]])
                if ki == len(ktiles) - 1:
                    fw.copy("act", osb[:, :n], po[pob][:, :n], reads=[Rpo[pob]], writes=[Rosb])
                    fw.copy("dve", denb[:, :n], osb[64:128, :n], reads=[Rosb], writes=[Rdenb])
                    fw.op("dve", lambda e, n=n: e.reciprocal(out=denb[:, :n], in_=denb[:, :n]), reads=[Rdenb], writes=[Rdenb])
                    fw.tt("dve", attnb[:, h, :n], osb[0:64, :n], denb[:, :n], ALU.mult, reads=[Rosb, Rdenb], pwrites=[Rattnb])

            prev = None
            for it in its:
                jp, half, h, pob, ki, kc, sb_, pb = it
                fw.mm(psc[sb_][:, :n], kTm[:, half, kc * 128:(kc + 1) * 128], qT[:, jp, :n], True, True,
                      reads=[RkT, RqT], pwrites=[Rpsc[sb_]])
                fw.act(pT[pb][:, :n], psc[sb_][:, :n], AF.Exp, scale=0.125, reads=[Rpsc[sb_]], writes=[RpT[pb]])
                if prev is not None:
                    emit_pv(prev)
                prev = it
            emit_pv(prev)
            for h in range(8):
                fw.act(sqb[:, h, :n], attnb[:, h, :n], AF.Square, reads=[Rattnb], pwrites=[Rsqb])
            for h in range(8):
                fw.mm(pprep[0][:, :n], cons[0:64, C_ONES:C_ONES + 128], sqb[:, h, :n], h == 0, h == 7, reads=[Rc, Rsqb], pwrites=[Rpp_[0]])
            fw.act(rs[:, :n], pprep[0][:, :n], AF.Ln, bias=EPS, scale=1.0 / 512, reads=[Rpp_[0]], writes=[Rrs])
            fw.act(rs[:, :n], rs[:, :n], AF.Exp, scale=-0.5, reads=[Rrs], writes=[Rrs])
            for h in range(8):
                fw.stt("dve", attn_n[:, h, :n], attnb[:, h, :n], ppc(pp, "aon", h)[0:64, :], rs[0:64, :n], ALU.mult, ALU.mult,
                       reads=[Rattnb, Rp, Rrs], pwrites=[Rattn_n])
            for i in range(n // 128):
                tb = tk % 2; tk += 1
                tsl = slice(i * 128, (i + 1) * 128)
                if r == 0:
                    src_ap, sR = xsrc.ap[t0 - 256 + i * 128:t0 - 256 + (i + 1) * 128, :], xsrc.R
                    dst_ap, dR = xdst.ap[t0 - 256 + i * 128:t0 - 256 + (i + 1) * 128, :], xdst.R
                else:
                    src_ap, sR = csrc.ap[i * 128:(i + 1) * 128, :], csrc.R
                    dst_ap, dR = cdst.ap[i * 128:(i + 1) * 128, :], cdst.R
                fw.dma("act", xt[tb][:], src_ap, reads=[sR], writes=[Rxt[tb]])
                for hf in range(2):
                    cs_ = slice(hf * 512, (hf + 1) * 512)
                    for h in range(8):
                        fw.mm(pso[hf][:], attn_n[:, h, tsl], woa[:, h, cs_], h == 0, False, reads=[Rattn_n, Rwo], pwrites=[Rpso[hf]])
                    for c in range(4):
                        fw.mm(pso[hf][:], sr[:, c, tsl], wob[:, c, cs_], False, c == 3, reads=[Rsr, Rwo], pwrites=[Rpso[hf]])
                    fw.tt("dve", ysb[tb][:, cs_], pso[hf][:], g1b[:, r, cs_], ALU.mult, reads=[Rpso[hf], Rg1], pwrites=[Rysb[tb]])
                fw.stt("dve", ysb[tb][:], xt[tb][:], ALPHA, ysb[tb][:], ALU.mult, ALU.add, reads=[Rxt[tb], Rysb[tb]], writes=[Rysb[tb]])
                ln_tile(fw, L, ysb[tb][:], Rysb[tb], dst_ap, dR)


def S5_moe(fw, g, l, xsrc, csrc, xdst, cdst, do_ctx, final=False):
    sets = [dict(N=4096, cap=512, src=xsrc, h2=g.h2x, acc=g.accx, dst=xdst, r=0, s0=0)]
    if do_ctx:
        sets.append(dict(N=256, cap=32, src=csrc, h2=g.h2c, acc=g.accc, dst=cdst, r=1, s0=512))
    NS = 544 if do_ctx else 512
    with fw.stage():
        cons, Rc, pp, Rp = load_common(fw, g, l)
        ident = cons[:, C_ID:C_ID + 128]
        idb = fw.sb("idb", [128, 128], BF16); Ridb = Res()
        fw.dma("pool", idb[:], g.consts.ap[:, C_ID:C_ID + 128], reads=[g.consts.R], writes=[Ridb])
        modb = fw.sb("modb", [128, 2, 3, 1024], F32); Rmodb = Res()
        for r in range(2 if do_ctx else 1):
            for k, which in enumerate((4, 3, 5)):
                fw.dma("sp", modb[:, r, k, :], g.modv.ap[l, r, which * 1024:(which + 1) * 1024].partition_broadcast(128),
                       reads=[g.modv.R], pwrites=[Rmodb])
            fw.ts("dve", modb[:, r, 0, :], modb[:, r, 0, :], 1.0, None, ALU.add, reads=[Rmodb], pwrites=[Rmodb])
        for s in sets:
            nsc = max(1, s["cap"] // 128)
            s["nsc"] = nsc
            s["ns"] = min(128, s["cap"])
            s["idxT"] = fw.sb("idxT", [128, nsc, 16], I32); s["valsT"] = fw.sb("valsT", [128, nsc, 16], F32); s["Rsel"] = Res()
        with fw.stage():
            wr = fw.sb("wr", [128, 8, 16], F32); Rwr = Res()
            for j in range(8):
                fw.dma("sp", wr[:, j, :], g.w_router.ap[l, j * 128:(j + 1) * 128, :], reads=[g.w_router.R], pwrites=[Rwr])
            xt = [fw.sb(f"axt{i}", [128, 1024], F32) for i in range(2)]; Rxt = [Res(), Res()]
            hb = [fw.sb(f"ahb{i}", [128, 1024], BF16) for i in range(2)]; Rhb = [Res(), Res()]
            hT = [fw.sb(f"ahT{i}", [128, 8, 128], F32) for i in range(2)]; RhT = [Res(), Res()]
            zer = fw.sb("azer", [128, 1024], F32); Rzer = Res()
            fw.memset("pool", zer[:], 0.0, writes=[Rzer])
            ptr = [fw.ps(f"aptr{i}", [128, 512]) for i in range(4)]; Rptr = [Res() for _ in range(4)]
            plg = [fw.ps(f"aplg{i}", [128, 512]) for i in range(2)]; Rplg = [Res(), Res()]
            pden = fw.ps("apden", [128, 512]); Rpden = Res()
            ptx = fw.ps("aptx", [128, 512]); Rptx = Res()
            for s in sets:
                N, cap, r = s["N"], s["cap"], s["r"]
                aff = fw.sb("aff", [16, N], F32); Raff = Res()
                work = fw.sb("awork", [16, N], F32); Rwork = Res()
                vals = fw.sb("avals", [16, cap], F32); Rvals = Res()
                idxu = fw.sb("aidxu", [16, cap], U32); Ridxu = Res()
                idxf = fw.sb("aidxf", [16, cap], F32); Ridxf = Res()
                den = fw.sb("aden", [16, 512], F32); Rden = Res()
                for tt in range(N // 128):
                    b = tt % 2
                    rows = slice(tt * 128, (tt + 1) * 128)
                    fw.dma("sp", xt[b][:], s["src"].ap[rows, :], reads=[s["src"].R], writes=[Rxt[b]])
                    fw.dma("act", s["acc"].ap[rows, :], zer[:], reads=[Rzer], pwrites=[s["acc"].R])
                    fw.tt("dve", xt[b][:], xt[b][:], modb[:, r, 0, :], ALU.mult, reads=[Rxt[b], Rmodb], writes=[Rxt[b]])
                    fw.tt("dve", xt[b][:], xt[b][:], modb[:, r, 1, :], ALU.add, reads=[Rxt[b], Rmodb], writes=[Rxt[b]])
                    fw.copy("act", hb[b][:], xt[b][:], reads=[Rxt[b]], writes=[Rhb[b]])
                    fw.dma("sp", s["h2"].ap[rows, :], hb[b][:], reads=[Rhb[b]], pwrites=[s["h2"].R])
                    for half in range(2):
                        pi = b * 2 + half
                        for jj in range(4):
                            j = half * 4 + jj
                            fw.tr(ptr[pi][:, jj * 128:(jj + 1) * 128], xt[b][:, j * 128:(j + 1) * 128], ident, reads=[Rxt[b], Rc], pwrites=[Rptr[pi]])
                        fw.copy("act", hT[b][:, half * 4:(half + 1) * 4, :].rearrange("p a b -> p (a b)"), ptr[pi][:], reads=[Rptr[pi]], pwrites=[RhT[b]])
                    for j in range(8):
                        fw.mm(plg[b][0:16, 0:128], wr[:, j, :], hT[b][:, j, :], j == 0, j == 7, reads=[Rwr, RhT[b]], pwrites=[Rplg[b]])
                    fw.act(aff[:, rows], plg[b][0:16, 0:128], AF.Exp, reads=[Rplg[b]], pwrites=[Raff])
                for c0 in range(0, N, 512):
                    n = min(512, N - c0)
                    fw.mm(pden[0:16, :n], cons[0:16, C_ONES:C_ONES + 16], aff[:, c0:c0 + n], True, True, reads=[Rc, Raff], pwrites=[Rpden])
                    fw.op("dve", lambda e, n=n, den=den: e.reciprocal(out=den[:, :n], in_=pden[0:16, :n]), reads=[Rpden], writes=[Rden])
                    fw.tt("dve", aff[:, c0:c0 + n], aff[:, c0:c0 + n], den[:, :n], ALU.mult, reads=[Raff, Rden], pwrites=[Raff])
                fw.copy("dve", work[:], aff[:], reads=[Raff], writes=[Rwork])
                for it in range(cap // 8):
                    sl8 = slice(it * 8, (it + 1) * 8)
                    fw.op("dve", lambda e, sl8=sl8, vals=vals, work=work: e.max(out=vals[:, sl8], in_=work[:]), reads=[Rwork], pwrites=[Rvals])
                    fw.op("dve", lambda e, sl8=sl8, vals=vals, work=work, idxu=idxu: e.max_index(out=idxu[:, sl8], in_max=vals[:, sl8], in_values=work[:]),
                          reads=[Rwork, Rvals], pwrites=[Ridxu])
                    fw.op("dve", lambda e, sl8=sl8, vals=vals, work=work: e.match_replace(out=work[:], in_to_replace=vals[:, sl8], in_values=work[:], imm_value=-1.0),
                          reads=[Rvals, Rwork], writes=[Rwork])
                fw.copy("dve", idxf[:], idxu[:], reads=[Ridxu], writes=[Ridxf])
                ns = s["ns"]
                for sc in range(s["nsc"]):
                    fw.tr(ptx[0:ns, 0:16], idxf[:, sc * 128:sc * 128 + ns], cons[0:16, C_ID:C_ID + 16], reads=[Ridxf, Rc], pwrites=[Rptx])
                    fw.tr(ptx[0:ns, 16:32], vals[:, sc * 128:sc * 128 + ns], cons[0:16, C_ID:C_ID + 16], reads=[Rvals, Rc], pwrites=[Rptx])
                    tmp = fw.sb("atmp", [128, 32], F32); Rtmp = Res()
                    fw.copy("act", tmp[0:ns, :], ptx[0:ns, 0:32], reads=[Rptx], writes=[Rtmp])
                    fw.copy("dve", s["idxT"][0:ns, sc, :], tmp[0:ns, 0:16], reads=[Rtmp], pwrites=[s["Rsel"]])
                    fw.copy("dve", s["valsT"][0:ns, sc, :], tmp[0:ns, 16:32], reads=[Rtmp], pwrites=[s["Rsel"]])
        with fw.stage():
            wgu = [fw.sb(f"wgu{i}", [128, 2, 8, 1024], BF16) for i in range(2)]; Rwgu = [Res(), Res()]
            wd = [fw.sb(f"wd{i}", [128, 8, 1024], BF16) for i in range(2)]; Rwd = [Res(), Res()]
            xs = [fw.sb(f"xs{i}", [128, 1024], BF16) for i in range(2)]; Rxs = [Res(), Res()]
            xsT = fw.sb("xsT", [128, 8, NS], BF16); RxsT = Res()
            gsb = [fw.sb(f"gsb{i}", [128, 512], F32) for i in range(2)]; Rgsb = [Res(), Res()]
            gsc = fw.sb("gsc", [128, 64], F32); Rgsc = Res()
            hidT = fw.sb("hidT", [128, 8, NS], BF16); RhidT = Res()
            ntl = 5 if do_ctx else 4
            yacc = fw.sb("yacc", [128, ntl, 1024], F32); Ryacc = [Res() for _ in range(ntl)]
            ysc = [fw.sb(f"ysc{i}", [128, 1024], F32) for i in range(2)]; Rysc = [Res(), Res()]
            ptb = fw.ps("bptb", [128, 1024], BF16); Rptb = Res()
            pg = [fw.ps(f"bpg{i}", [128, 512]) for i in range(2)]; Rpg = [Res(), Res()]
            pu = [fw.ps(f"bpu{i}", [128, 512]) for i in range(2)]; Rpu = [Res(), Res()]
            pc = fw.ps("bpc", [128, 512]); Rpc = Res()
            pyy = [fw.ps(f"bpy{i}", [128, 512]) for i in range(2)]; Rpyy = [Res(), Res()]
            tiles = []
            for s in sets:
                for sc in range(s["nsc"]):
                    tiles.append((s, sc, s["ns"], s["s0"] + sc * 128))
            deferred = []
            xk = 0
            gk = 0
            yk = 0
            for e in range(16):
                for (s, sc, ns, sl0) in tiles:
                    b = xk % 2; xk += 1
                    fw.dma_custom("pool", lambda en, s=s, sc=sc, ns=ns, b=b, e=e: en.indirect_dma_start(
                        out=xs[b][0:ns, :], out_offset=None, in_=s["h2"].ap,
                        in_offset=bass.IndirectOffsetOnAxis(ap=s["idxT"][0:ns, sc, e:e + 1], axis=0)),
                        reads=[s["h2"].R, s["Rsel"]], writes=[Rxs[b]])
                    for j in range(8):
                        fw.tr(ptb[:, j * 128:j * 128 + ns], xs[b][0:ns, j * 128:(j + 1) * 128], idb[0:ns, 0:ns], reads=[Rxs[b], Ridb], pwrites=[Rptb])
                    fw.copy("act", xsT[:, :, sl0:sl0 + ns], ptb[:].rearrange("p (j s) -> p j s", j=8)[:, :, 0:ns], reads=[Rptb], pwrites=[RxsT])
                for fh in range(2):
                    wb = fh
                    for j in range(8):
                        rows = slice(j * 128, (j + 1) * 128)
                        cols = slice(fh * 1024, (fh + 1) * 1024)
                        wga, wgR = g.moe_w("gate", l, e)
                        wua, wuR = g.moe_w("up", l, e)
                        wda, wdR = g.moe_w("down", l, e)
                        fw.dma("pool", wgu[wb][:, 0, j, :], wga[rows, cols], reads=[wgR], pwrites=[Rwgu[wb]])
                        fw.dma("pool", wgu[wb][:, 1, j, :], wua[rows, cols], reads=[wuR], pwrites=[Rwgu[wb]])
                        fw.dma("pool", wd[wb][:, j, :], wda[fh * 1024 + j * 128:fh * 1024 + (j + 1) * 128, :], reads=[wdR], pwrites=[Rwd[wb]])
                    if fh == 0:
                        for f in deferred:
                            f()
                        deferred = []
                    for fc in range(8):
                        b = gk % 2; gk += 1
                        fcs = slice(fc * 128, (fc + 1) * 128)
                        for j in range(8):
                            fw.mm(pg[b][:], wgu[wb][:, 0, j, fcs], xsT[:, j, 0:512], j == 0, j == 7, reads=[Rwgu[wb], RxsT], pwrites=[Rpg[b]])
                        for j in range(8):
                            fw.mm(pu[b][:], wgu[wb][:, 1, j, fcs], xsT[:, j, 0:512], j == 0, j == 7, reads=[Rwgu[wb], RxsT], pwrites=[Rpu[b]])
                        fw.act(gsb[b][:], pg[b][:], AF.Silu, reads=[Rpg[b]], writes=[Rgsb[b]])
                        fw.tt("dve", hidT[:, fc, 0:512], gsb[b][:], pu[b][:], ALU.mult, reads=[Rgsb[b], Rpu[b]], pwrites=[RhidT])
                        if do_ctx:
                            for j in range(8):
                                fw.mm(pc[:, 0:32], wgu[wb][:, 0, j, fcs], xsT[:, j, 512:544], j == 0, j == 7, reads=[Rwgu[wb], RxsT], pwrites=[Rpc])
                            for j in range(8):
                                fw.mm(pc[:, 32:64], wgu[wb][:, 1, j, fcs], xsT[:, j, 512:544], j == 0, j == 7, reads=[Rwgu[wb], RxsT], pwrites=[Rpc])
                            fw.copy("act", gsc[:, 0:64], pc[:, 0:64], reads=[Rpc], writes=[Rgsc])
                            fw.act(gsc[:, 0:32], gsc[:, 0:32], AF.Silu, reads=[Rgsc], writes=[Rgsc])
                            fw.tt("dve", hidT[:, fc, 512:544], gsc[:, 0:32], gsc[:, 32:64], ALU.mult, reads=[Rgsc], pwrites=[RhidT])
                    for ti, (s, sc, ns, sl0) in enumerate(tiles):
                        for hf in range(2):
                            yb = yk % 2; yk += 1
                            cs_ = slice(hf * 512, (hf + 1) * 512)
                            for fc in range(8):
                                fw.mm(pyy[yb][0:ns, :], hidT[:, fc, sl0:sl0 + ns], wd[wb][:, fc, cs_], fc == 0, fc == 7,
                                      reads=[RhidT, Rwd[wb]], pwrites=[Rpyy[yb]])
                            if fh == 0:
                                fw.copy("act", yacc[0:ns, ti, cs_], pyy[yb][0:ns, :], reads=[Rpyy[yb]], pwrites=[Ryacc[ti]])
                            else:
                                fw.tt("dve", yacc[0:ns, ti, cs_], pyy[yb][0:ns, :], yacc[0:ns, ti, cs_], ALU.add, reads=[Rpyy[yb], Ryacc[ti]], pwrites=[Ryacc[ti]])
                for ti, (s, sc, ns, sl0) in enumerate(tiles):
                    def sc_fn(s=s, sc=sc, ns=ns, ti=ti, e=e):
                        b = sc_fn.k[0] % 2; sc_fn.k[0] += 1
                        fw.ts("dve", ysc[b][0:ns, :], yacc[0:ns, ti, :], s["valsT"][0:ns, sc, e:e + 1], None, ALU.mult,
                              reads=[Ryacc[ti], s["Rsel"]], writes=[Rysc[b]])
                        fw.dma_custom("pool", lambda en: en.indirect_dma_start(
                            out=s["acc"].ap, out_offset=bass.IndirectOffsetOnAxis(ap=s["idxT"][0:ns, sc, e:e + 1], axis=0),
                            in_=ysc[b][0:ns, :], in_offset=None, compute_op=ALU.add),
                            reads=[Rysc[b], s["Rsel"]], writes=[s["acc"].R])
                    sc_fn.k = S5_moe._k
                    deferred.append(sc_fn)
            for f in deferred:
                f()
        with fw.stage():
            L = LNCtx(fw, g, l, "ln2_w", "ln2_b")
            xt = [fw.sb(f"cxt{i}", [128, 1024], F32) for i in range(2)]; Rxt = [Res(), Res()]
            at = [fw.sb(f"cat{i}", [128, 1024], F32) for i in range(2)]; Rat = [Res(), Res()]
            k = 0
            for s in sets:
                for tt in range(s["N"] // 128):
                    b = k % 2; k += 1
                    rows = slice(tt * 128, (tt + 1) * 128)
                    fw.dma("sp", xt[b][:], s["src"].ap[rows, :], reads=[s["src"].R], writes=[Rxt[b]])
                    fw.dma("act", at[b][:], s["acc"].ap[rows, :], reads=[s["acc"].R], writes=[Rat[b]])
                    fw.tt("dve", at[b][:], at[b][:], modb[:, s["r"], 2, :], ALU.mult, reads=[Rat[b], Rmodb], writes=[Rat[b]])
                    fw.stt("dve", at[b][:], xt[b][:], ALPHA, at[b][:], ALU.mult, ALU.add, reads=[Rxt[b], Rat[b]], writes=[Rat[b]])
                    ln_tile(fw, L, at[b][:], Rat[b], s["dst"].ap[rows, :], s["dst"].R, is_output=(final and s["r"] == 0))


S5_moe._k = [0]


def pipeline(fw, g, out):
    S0_mod(fw, g)
    S1_inproj(fw, g, 0, g.x, g.ctx)
    S2_rg(fw, g, 0)
    S3_ssd(fw, g, 0)
    S4_attn(fw, g, 0, g.x, g.ctx, g.x1, g.ctx1, True)
    S5_moe(fw, g, 0, g.x1, g.ctx1, g.xa, g.ctxa, True)
    S1_inproj(fw, g, 1, g.xa, g.ctxa)
    S2_rg(fw, g, 1)
    S3_ssd(fw, g, 1)
    S4_attn(fw, g, 1, g.xa, g.ctxa, g.x1, None, False)
    S5_moe(fw, g, 1, g.x1, None, out, None, False, final=True)

def kernel(x, c, ctx, c_ctx, w_mod, b_mod, w_in, q_norm, k_norm, attn_out_norm,
           ssd_conv_w, ssd_conv_b, ssd_dt_bias, ssd_a_log, ssd_d, ssd_norm,
           rg_conv_w, rg_conv_b, rg_wa, rg_ba, rg_wx, rg_bx, rg_lambda, rg_out_norm,
           w_out, ln1_w, ln1_b, w_router, w_gate, w_up, w_down, ln2_w, ln2_b):
    f = lambda a: np.ascontiguousarray(np.asarray(a, dtype=np.float32))
    small = dict(q_norm=f(q_norm), k_norm=f(k_norm), attn_out_norm=f(attn_out_norm), ssd_conv_w=f(ssd_conv_w),
                 ssd_conv_b=f(ssd_conv_b), ssd_d=f(ssd_d), ssd_norm=f(ssd_norm), rg_conv_w=f(rg_conv_w),
                 rg_conv_b=f(rg_conv_b), rg_ba=f(rg_ba), rg_bx=f(rg_bx), rg_lambda=f(rg_lambda), rg_out_norm=f(rg_out_norm))
    shared = dict(b_mod=f(b_mod), ssd_dt_bias=f(ssd_dt_bias).reshape(2, 8),
                  ssd_a_log=f(ssd_a_log).reshape(2, 8), rg_wa=f(rg_wa), rg_wx=f(rg_wx), w_out=f(w_out),
                  ln1_w=f(ln1_w), ln1_b=f(ln1_b), w_router=f(w_router), ln2_w=f(ln2_w), ln2_b=f(ln2_b),
                  pp=np.stack([host_pp(small, l) for l in range(2)]), consts=host_consts(), rope=host_rope())
    w_mod = f(w_mod); w_in = f(w_in); w_gate = f(w_gate); w_up = f(w_up); w_down = f(w_down)
    for l in range(2):
        shared[f"w_in{l}"] = w_in[l]
        for h in range(2):
            shared[f"w_mod{l}_{h}"] = w_mod[l, h * 512:(h + 1) * 512]
        for e in range(16):
            shared[f"wg{l}_{e}"] = w_gate[l, e]
            shared[f"wu{l}_{e}"] = w_up[l, e]
            shared[f"wd{l}_{e}"] = w_down[l, e]
    x = f(x); ctx = f(ctx); c = f(c); c_ctx = f(c_ctx)
    nb = x.shape[0]
    nc = bass.Bass("TRN2", target_bir_lowering=False)
    with ExitStack() as es:
        fw = FW(nc, es)
        g = make_G(nc)
        out = DT(nc, "out", [4096, 1024], F32, kind="ExternalOutput")
        pipeline(fw, g, out)
        fw.finish()
    in_maps = []
    for b in range(nb):
        cc = np.zeros((128, 16), np.float32)
        cc[:, 0::2] = c[b].reshape(8, 128).T
        cc[:, 1::2] = c_ctx.reshape(8, 128).T
        in_maps.append(dict(shared, x=x[b], ctx=ctx[b], cc=cc))
    res = run_bass_kernel_spmd(nc, in_maps, core_ids=list(range(nb)))
    return np.stack([np.asarray(r["out"], dtype=np.float32) for r in res.results], axis=0)
```

```python
from concourse.bass_utils import run_bass_kernel_spmd
import numpy as np
import concourse.bass as bass
import concourse.mybir as mybir
from contextlib import ExitStack

F32 = mybir.dt.float32
BF16 = mybir.dt.bfloat16
I32 = mybir.dt.int32
U32 = mybir.dt.uint32
AF = mybir.ActivationFunctionType
ALU = mybir.AluOpType
AX = mybir.AxisListType


class Res:
    __slots__ = ("name", "w", "r")

    def __init__(self, name=""):
        self.name = name
        self.w = []
        self.r = []


class FW:
    ENG = ("pe", "act", "dve", "pool", "sp")
    EPOCH = 30000
    NDMA = 12

    def __init__(self, nc, es):
        self.nc = nc
        self.es = es
        self.q = {e: [] for e in self.ENG}
        self.sems = {}
        self.cnt = {}
        self.cur = {}
        self.known = {e: {} for e in self.ENG}
        self.epoch = {e: 0 for e in self.ENG}
        for e in self.ENG:
            self._new_eng_sem(e)
        self.dma_pool = {}
        self.dma_rr = {}
        self.out_events = []
        self.n_inst = 0
        self.uid = 0
        self.tes = es

    def _sem(self, key):
        if key not in self.sems:
            self.sems[key] = self.es.enter_context(self.nc.semaphore("s_" + key))
            self.cnt[key] = 0
        return self.sems[key]

    def _new_eng_sem(self, e):
        key = f"{e}{self.epoch[e]}"
        self.epoch[e] += 1
        self._sem(key)
        self.cur[e] = key

    def sb(self, name, shape, dt):
        self.uid += 1
        return self.tes.enter_context(self.nc.sbuf_tensor(f"{name}_{self.uid}", list(shape), dt))

    def ps(self, name, shape, dt=F32):
        self.uid += 1
        return self.tes.enter_context(self.nc.psum_tensor(f"{name}_{self.uid}", list(shape), dt))

    def stage(self):
        fw = self

        class _S:
            def __enter__(s2):
                fw.barrier()
                s2.old = fw.tes
                s2.st = ExitStack()
                fw.tes = s2.st
                return s2

            def __exit__(s2, *a):
                fw.barrier()
                s2.st.close()
                fw.tes = s2.old
                return False
        return _S()

    def barrier(self):
        targets = [(k, c) for k, c in self.cnt.items() if c > 0]
        for eng in self.ENG:
            kn = self.known[eng]
            wl = []
            for k, c in targets:
                if kn.get(k, 0) < c:
                    kn[k] = c
                    wl.append((self.sems[k], c))
            if wl:
                def emit(e, wl=wl):
                    for s, v in wl:
                        e.wait_ge(s, v)
                self.q[eng].append(emit)

    def mm(self, out, lhsT, rhs, start, stop, reads=(), pwrites=()):
        return self.op("pe", lambda e: e.matmul(out, lhsT=lhsT, rhs=rhs, start=start, stop=stop), reads=reads, pwrites=pwrites)

    def tr(self, out, in_, ident, reads=(), pwrites=()):
        return self.op("pe", lambda e: e.transpose(out, in_, ident), reads=reads, pwrites=pwrites)

    def act(self, out, in_, func, reads=(), writes=(), pwrites=(), **kw):
        return self.op("act", lambda e: e.activation(out=out, in_=in_, func=func, **kw), reads=reads, writes=writes, pwrites=pwrites)

    def copy(self, eng, out, in_, reads=(), writes=(), pwrites=()):
        if eng == "act":
            return self.op("act", lambda e: e.activation(out=out, in_=in_, func=AF.Copy), reads=reads, writes=writes, pwrites=pwrites)
        return self.op(eng, lambda e: e.tensor_copy(out=out, in_=in_), reads=reads, writes=writes, pwrites=pwrites)

    def tt(self, eng, out, in0, in1, op, reads=(), writes=(), pwrites=()):
        return self.op(eng, lambda e: e.tensor_tensor(out=out, in0=in0, in1=in1, op=op), reads=reads, writes=writes, pwrites=pwrites)

    def ts(self, eng, out, in0, s1, s2, op0, op1=None, reads=(), writes=(), pwrites=()):
        if op1 is None:
            return self.op(eng, lambda e: e.tensor_scalar(out=out, in0=in0, scalar1=s1, scalar2=None, op0=op0), reads=reads, writes=writes, pwrites=pwrites)
        return self.op(eng, lambda e: e.tensor_scalar(out=out, in0=in0, scalar1=s1, scalar2=s2, op0=op0, op1=op1), reads=reads, writes=writes, pwrites=pwrites)

    def stt(self, eng, out, in0, scalar, in1, op0, op1, reads=(), writes=(), pwrites=()):
        return self.op(eng, lambda e: e.scalar_tensor_tensor(out=out, in0=in0, scalar=scalar, in1=in1, op0=op0, op1=op1), reads=reads, writes=writes, pwrites=pwrites)

    def memset(self, eng, ap, val, reads=(), writes=(), pwrites=()):
        return self.op(eng, lambda e: e.memset(ap, val), reads=reads, writes=writes, pwrites=pwrites)

    def _need(self, eng, reads, writes, pwrites=(), cls=None):
        need = {}
        if cls is None:
            cls = eng

        def add(ev, skip_same_pe=False):
            k, v, src = ev
            if skip_same_pe and src == "pe" and eng == "pe":
                return
            if need.get(k, 0) < v:
                need[k] = v
        for r in reads:
            for ev in r.w:
                add(ev)
        for w in writes:
            for ev in w.w:
                add(ev, skip_same_pe=True)
            for ev in w.r:
                add(ev)
        for w in pwrites:
            for ev in w.r:
                add(ev)
            for ev in w.w:
                if ev[2] != cls:
                    add(ev)
        out = []
        kn = self.known[eng]
        for k, v in need.items():
            if kn.get(k, 0) < v:
                kn[k] = v
                out.append((k, v))
        return out

    def _record(self, ev, reads, writes, pwrites=()):
        for r in reads:
            r.r = [e for e in r.r if e[0] != ev[0]] + [ev]
        for w in writes:
            w.w = [ev]
            w.r = []
        for w in pwrites:
            w.w = [e for e in w.w if e[0] != ev[0]] + [ev]

    def op(self, eng, fn, reads=(), writes=(), pwrites=()):
        reads = [r for r in reads if r is not None]
        writes = [w for w in writes if w is not None]
        pwrites = [w for w in pwrites if w is not None]
        waits = self._need(eng, reads, writes, pwrites)
        key = self.cur[eng]
        self.cnt[key] += 1
        val = self.cnt[key]
        sem = self.sems[key]
        wl = [(self.sems[k], v) for k, v in waits]

        def emit(e, fn=fn, wl=wl, sem=sem):
            for s, v in wl:
                e.wait_ge(s, v)
            fn(e).then_inc(sem, 1)
        self.q[eng].append(emit)
        self.n_inst += 1
        ev = (key, val, eng)
        self._record(ev, reads, writes, pwrites)
        if val >= self.EPOCH:
            self._new_eng_sem(eng)
        return ev

    def dma(self, queue, out, in_, reads=(), writes=(), pwrites=(), is_output=False, **kw):
        reads = [r for r in reads if r is not None]
        writes = [w for w in writes if w is not None]
        pwrites = [w for w in pwrites if w is not None]
        pool = self.dma_pool.setdefault(queue, [f"d{queue}{i}" for i in range(self.NDMA)])
        i = self.dma_rr.get(queue, 0)
        self.dma_rr[queue] = (i + 1) % len(pool)
        key = pool[i]
        sem = self._sem(key)
        if self.cnt[key] > 30000:
            nk = key + "n"
            pool[i] = nk
            prev_key, prev_val = key, self.cnt[key]
            key = nk
            sem = self._sem(key)
            extra = [(prev_key, prev_val)]
        else:
            extra = [(key, self.cnt[key])] if self.cnt[key] > 0 else []
        waits = self._need(queue, reads, writes, pwrites, cls='dma')
        kn = self.known[queue]
        for k, v in extra:
            if kn.get(k, 0) < v:
                kn[k] = v
                waits.append((k, v))
        self.cnt[key] += 16
        val = self.cnt[key]
        wl = [(self.sems[k], v) for k, v in waits]

        def emit(e, out=out, in_=in_, wl=wl, sem=sem, kw=kw):
            for s, v in wl:
                e.wait_ge(s, v)
            e.dma_start(out=out, in_=in_, **kw).then_inc(sem, 16)
        self.q[queue].append(emit)
        self.n_inst += 1
        ev = (key, val, "dma")
        self._record(ev, reads, writes, pwrites)
        if is_output:
            self.out_events.append(ev)
        return ev

    def dma_custom(self, queue, fn, reads=(), writes=(), pwrites=(), is_output=False):
        reads = [r for r in reads if r is not None]
        writes = [w for w in writes if w is not None]
        pwrites = [w for w in pwrites if w is not None]
        pool = self.dma_pool.setdefault(queue, [f"d{queue}{i}" for i in range(self.NDMA)])
        i = self.dma_rr.get(queue, 0)
        self.dma_rr[queue] = (i + 1) % len(pool)
        key = pool[i]
        sem = self._sem(key)
        extra = [(key, self.cnt[key])] if self.cnt[key] > 0 else []
        waits = self._need(queue, reads, writes, pwrites, cls='dma')
        kn = self.known[queue]
        for k, v in extra:
            if kn.get(k, 0) < v:
                kn[k] = v
                waits.append((k, v))
        self.cnt[key] += 16
        val = self.cnt[key]
        wl = [(self.sems[k], v) for k, v in waits]

        def emit(e, fn=fn, wl=wl, sem=sem):
            for s, v in wl:
                e.wait_ge(s, v)
            fn(e).then_inc(sem, 16)
        self.q[queue].append(emit)
        self.n_inst += 1
        ev = (key, val, "dma")
        self._record(ev, reads, writes, pwrites)
        if is_output:
            self.out_events.append(ev)
        return ev

    def finish(self):
        final_waits = [(self.sems[k], v) for (k, v, _) in self.out_events]
        q = self.q
        with self.nc.Block() as block:
            @block.tensor
            def _(e):
                for f in q["pe"]:
                    f(e)

            @block.scalar
            def _(e):
                for f in q["act"]:
                    f(e)

            @block.vector
            def _(e):
                for f in q["dve"]:
                    f(e)

            @block.gpsimd
            def _(e):
                for f in q["pool"]:
                    f(e)

            @block.sync
            def _(e):
                for f in q["sp"]:
                    f(e)
                for s, v in final_waits:
                    e.wait_ge(s, v)


D = 1024
NT = 4352
NTT = 34
BLOCKS = [(0, 256)] + [(256 + i * 512, 512) for i in range(8)]
EPS = 1e-6
ALPHA = 4 ** 0.25
C_ID, C_U, C_UT, C_NMF, C_NMB, C_ONES, C_BONES, C_RT = [i * 128 for i in range(8)]
PP = {}
_o = 0
for _n, _w in [("qw", 1), ("kw", 1), ("aon", 8), ("scw", 16), ("scb", 4), ("sd", 2), ("sn", 2),
               ("rcw", 8), ("rcb", 2), ("rba", 4), ("rbx", 4), ("rlam", 4), ("rn", 2)]:
    PP[_n] = (_o, _w)
    _o += _w
NPP = _o


def host_consts():
    c = np.zeros((128, 1024), np.float32)
    k = np.arange(128)[:, None]
    m = np.arange(128)[None, :]
    c[:, C_ID:C_ID + 128] = np.eye(128)
    c[:, C_U:C_U + 128] = (k <= m)
    c[:, C_UT:C_UT + 128] = (k >= m)
    c[:, C_NMF:C_NMF + 128] = np.where(k <= m, 0.0, -1e4)
    c[:, C_NMB:C_NMB + 128] = np.where(k >= m, 0.0, -1e4)
    c[:, C_ONES:C_ONES + 128] = 1.0
    c[:, C_BONES:C_BONES + 128] = ((k // 64) == (m // 64))
    rt = np.zeros((128, 128), np.float32)
    for base in (0, 64):
        for part in (0, 32):
            for d in range(16):
                mm_ = base + part + d
                rt[mm_ + 16, mm_] = -1.0
                rt[mm_, mm_ + 16] = 1.0
    c[:, C_RT:C_RT + 128] = rt
    return c


def host_rope():
    n = 4096
    t = np.arange(n)
    row = (t // 64).astype(np.float32)
    col = (t % 64).astype(np.float32)
    inv = (10000.0 ** (-np.arange(0, 32, 2, dtype=np.float32) / 32)).astype(np.float32)
    ang_r = row[:, None] * inv[None, :]
    ang_c = col[:, None] * inv[None, :]
    out = np.zeros((128, 2, n), np.float32)
    for p in range(128):
        d = p % 64
        a = ang_r if d < 32 else ang_c
        f = d % 16
        out[p, 0] = np.cos(a[:, f])
        out[p, 1] = np.sin(a[:, f])
    return out.reshape(128, 2 * n)


def host_pp(inp, l):
    pp = np.zeros((128, NPP), np.float32)

    def put(name, arr):
        o, w = PP[name]
        pp[:arr.shape[0], o:o + w] = arr.reshape(arr.shape[0], w)
    put("qw", np.tile(inp["q_norm"][l], 2)[:, None])
    put("kw", np.tile(inp["k_norm"][l], 2)[:, None])
    put("aon", inp["attn_out_norm"][l].reshape(8, 64).T)
    put("scw", inp["ssd_conv_w"][l].reshape(4, 4, 128).transpose(2, 1, 0))
    put("scb", inp["ssd_conv_b"][l].reshape(4, 128).T)
    put("sd", np.repeat(inp["ssd_d"][l], 64).reshape(2, 128).T)
    put("sn", inp["ssd_norm"][l].reshape(2, 128).T)
    put("rcw", inp["rg_conv_w"][l].reshape(4, 2, 128).transpose(2, 1, 0))
    put("rcb", inp["rg_conv_b"][l].reshape(2, 128).T)
    put("rba", inp["rg_ba"][l].reshape(2, 2, 128).transpose(2, 0, 1))
    put("rbx", inp["rg_bx"][l].reshape(2, 2, 128).transpose(2, 0, 1))
    put("rlam", inp["rg_lambda"][l].reshape(2, 2, 128).transpose(2, 0, 1))
    put("rn", inp["rg_out_norm"][l].reshape(2, 128).T)
    return pp


class DT:
    def __init__(self, nc, name, shape, dt, kind="Internal"):
        self.h = nc.dram_tensor(name, list(shape), dt, kind=kind)
        self.ap = self.h.ap()
        self.R = Res(name)
        self.name = name


IN_SHAPES = {
    "x": ([4096, 1024], F32), "ctx": ([256, 1024], F32), "cc": ([128, 16], F32),
    "b_mod": ([2, 6144], F32),
    "ssd_dt_bias": ([2, 8], F32), "ssd_a_log": ([2, 8], F32),
    "rg_wa": ([2, 2, 4, 64, 64], F32), "rg_wx": ([2, 2, 4, 64, 64], F32),
    "w_out": ([2, 1024, 1024], F32), "ln1_w": ([2, 1024], F32), "ln1_b": ([2, 1024], F32),
    "w_router": ([2, 1024, 16], F32), "ln2_w": ([2, 1024], F32), "ln2_b": ([2, 1024], F32),
    "pp": ([2, 128, NPP], F32), "consts": ([128, 1024], F32), "rope": ([128, 8192], F32),
}
for _l in range(2):
    IN_SHAPES[f"w_in{_l}"] = ([1024, 2056], F32)
    for _h in range(2):
        IN_SHAPES[f"w_mod{_l}_{_h}"] = ([512, 6144], F32)
    for _e in range(16):
        IN_SHAPES[f"wg{_l}_{_e}"] = ([1024, 2048], F32)
        IN_SHAPES[f"wu{_l}_{_e}"] = ([1024, 2048], F32)
        IN_SHAPES[f"wd{_l}_{_e}"] = ([2048, 1024], F32)
SCRATCH = {
    "modv": ([2, 2, 6144], F32), "projT": ([2056, NT], F32), "vtok": ([NT, 128], F32), "dttok": ([NT, 8], F32),
    "mergedT": ([1024, NT], BF16), "x1": ([4096, 1024], F32), "ctx1": ([256, 1024], F32),
    "xa": ([4096, 1024], F32), "ctxa": ([256, 1024], F32),
    "h2x": ([4096, 1024], BF16), "h2c": ([256, 1024], BF16), "accx": ([4096, 1024], F32), "accc": ([256, 1024], F32),
}


class G:
    gathered = False

    def moe_w(self, kind, l, e):
        t = getattr(self, {"gate": "wg", "up": "wu", "down": "wd"}[kind] + f"{l}_{e}")
        return t.ap, t.R


def make_G(nc, ext_in=(), ext_out=(), only=None):
    g = G()
    for n, (sh, dt) in IN_SHAPES.items():
        if only is not None and n not in only:
            continue
        setattr(g, n, DT(nc, n, sh, dt, kind="ExternalInput"))
    for n, (sh, dt) in SCRATCH.items():
        kind = "ExternalInput" if n in ext_in else ("ExternalOutput" if n in ext_out else "Internal")
        if only is not None and kind == "Internal" and n not in only:
            continue
        setattr(g, n, DT(nc, n, sh, dt, kind=kind))
    return g


def S0_mod(fw, g):
    with fw.stage():
        cc = fw.sb("cc", [128, 16], F32); Rcc = Res()
        sc = fw.sb("sc", [128, 16], F32); Rsc = Res()
        fw.dma("sp", cc[:], g.cc.ap, reads=[g.cc.R], writes=[Rcc])
        fw.act(sc[:], cc[:], AF.Silu, reads=[Rcc], writes=[Rsc])
        wm = [fw.sb(f"wm{i}", [128, 8, 512], F32) for i in range(2)]; Rwm = [Res(), Res()]
        bm = fw.sb("bm", [2, 6144], F32); Rbm = Res()
        mo = fw.sb("mo", [2, 6144], F32); Rmo = Res()
        ps = [fw.ps(f"mps{i}", [2, 512]) for i in range(2)]; Rps = [Res(), Res()]
        for l in range(2):
            for r in range(2):
                fw.dma("act", bm[r:r + 1, :], g.b_mod.ap[l:l + 1, :], reads=[g.b_mod.R], pwrites=[Rbm])
            wvs = [getattr(g, f"w_mod{l}_{h}") for h in range(2)]
            for n in range(12):
                b = n % 2
                for h in range(2):
                    fw.dma("sp", wm[b][:, h * 4:(h + 1) * 4, :], wvs[h].ap.rearrange("(j p) n -> p j n", p=128)[:, :, n * 512:(n + 1) * 512],
                           reads=[wvs[h].R], writes=[Rwm[b]] if h == 0 else [], pwrites=[] if h == 0 else [Rwm[b]])
                for j in range(8):
                    fw.mm(ps[b][:], sc[:, 2 * j:2 * j + 2], wm[b][:, j, :], j == 0, j == 7, reads=[Rsc, Rwm[b]], pwrites=[Rps[b]])
                fw.tt("dve", mo[:, n * 512:(n + 1) * 512], ps[b][:], bm[:, n * 512:(n + 1) * 512], ALU.add,
                      reads=[Rps[b], Rbm], pwrites=[Rmo])
            fw.dma("sp", g.modv.ap[l], mo[:], reads=[Rmo], pwrites=[g.modv.R])


def load_modcols(fw, g, l, which, dst, Rdst, eng="sp"):
    for r in range(2):
        src = g.modv.ap[l, r, which * 1024:(which + 1) * 1024].rearrange("(j p) -> p j", p=128)
        fw.dma(eng, dst[:, r, :], src, reads=[g.modv.R], pwrites=[Rdst], allow_slow_non_contiguous=True)


FCHUNKS = [(c * 128, 128) for c in range(12)] + [(1536, 8), (1544, 128), (1672, 128), (1800, 128), (1928, 128)]


def S1_inproj(fw, g, l, xsrc, csrc):
    with fw.stage():
        cons = fw.sb("cons", [128, 128], F32); Rcons = Res()
        fw.dma("sp", cons[:], g.consts.ap[:, C_ID:C_ID + 128], reads=[g.consts.R], writes=[Rcons])
        msc = fw.sb("msc", [128, 2, 8], F32); msh = fw.sb("msh", [128, 2, 8], F32); Rm = Res()
        load_modcols(fw, g, l, 0, msh, Rm)
        load_modcols(fw, g, l, 1, msc, Rm)
        fw.ts("dve", msc[:], msc[:], 1.0, None, ALU.add, reads=[Rm], pwrites=[Rm])
        win = fw.sb("win", [128, 8, 2056], BF16); Rwin = Res()
        for j in range(8):
            wi = getattr(g, f"w_in{l}")
            fw.dma("pool", win[:, j, :], wi.ap[j * 128:(j + 1) * 128, :], reads=[wi.R], pwrites=[Rwin])
        hxT = fw.sb("hxT", [128, 8, NT], BF16); RhxT = [Res() for _ in range(NTT)]
        xts = [fw.sb(f"xt{i}", [128, 1024], F32) for i in range(2)]; Rxt = [Res(), Res()]
        tps = [fw.ps(f"tp{i}", [128, 512]) for i in range(4)]; Rtp = [Res() for _ in range(4)]
        for tt in range(NTT):
            if tt < 2:
                src, sR, s = csrc.ap[tt * 128:(tt + 1) * 128, :], csrc.R, 1
            else:
                src, sR, s = xsrc.ap[(tt - 2) * 128:(tt - 1) * 128, :], xsrc.R, 0
            xt = xts[tt % 2]
            fw.dma("sp", xt[:], src, reads=[sR], writes=[Rxt[tt % 2]])
            for half in range(2):
                pi = (tt % 2) * 2 + half
                for jj in range(4):
                    j = half * 4 + jj
                    fw.tr(tps[pi][:, jj * 128:(jj + 1) * 128], xt[:, j * 128:(j + 1) * 128], cons[:],
                          reads=[Rxt[tt % 2], Rcons], pwrites=[Rtp[pi]])
                for jj in range(4):
                    j = half * 4 + jj
                    fw.act(hxT[:, j, tt * 128:(tt + 1) * 128], tps[pi][:, jj * 128:(jj + 1) * 128], AF.Identity,
                           bias=msh[:, s, j:j + 1], scale=msc[:, s, j:j + 1], reads=[Rtp[pi], Rm], pwrites=[RhxT[tt]])
        pps = [fw.ps(f"pp{i}", [128, 512]) for i in range(3)]; Rpp = [Res() for _ in range(3)]
        stg = [fw.sb(f"stg{i}", [128, 512], F32) for i in range(3)]; Rst = [Res() for _ in range(3)]
        k = 0
        for (t0, n) in BLOCKS:
            Rh = RhxT[t0 // 128:(t0 + n) // 128]
            for (c0, w) in FCHUNKS:
                b = k % 3
                for j in range(8):
                    fw.mm(pps[b][:w, :n], win[:, j, c0:c0 + w], hxT[:, j, t0:t0 + n], j == 0, j == 7,
                          reads=[Rwin] + Rh, pwrites=[Rpp[b]])
                fw.copy("act" if k % 2 == 0 else "dve", stg[b][:w, :n], pps[b][:w, :n], reads=[Rpp[b]], writes=[Rst[b]])
                fw.dma("sp" if k % 2 == 0 else "act", g.projT.ap[c0:c0 + w, t0:t0 + n], stg[b][:w, :n],
                       reads=[Rst[b]], pwrites=[g.projT.R])
                k += 1
        pv = fw.ps("pv", [128, 136]); Rpv = Res()
        vst = [fw.sb(f"vst{i}", [128, 136], F32) for i in range(2)]; Rvst = [Res(), Res()]
        for tt in range(NTT):
            sl = slice(tt * 128, (tt + 1) * 128)
            for j in range(8):
                fw.mm(pv[:, 0:128], hxT[:, j, sl], win[:, j, 640:768], j == 0, j == 7, reads=[Rwin, RhxT[tt]], pwrites=[Rpv])
            for j in range(8):
                fw.mm(pv[:, 128:136], hxT[:, j, sl], win[:, j, 1536:1544], j == 0, j == 7, reads=[Rwin, RhxT[tt]], pwrites=[Rpv])
            b = tt % 2
            fw.copy("dve", vst[b][:], pv[:], reads=[Rpv], writes=[Rvst[b]])
            fw.dma("sp", g.vtok.ap[sl, :], vst[b][:, 0:128], reads=[Rvst[b]], pwrites=[g.vtok.R])
            fw.dma("act", g.dttok.ap[sl, :], vst[b][:, 128:136], reads=[Rvst[b]], pwrites=[g.dttok.R])


def load_common(fw, g, l):
    cons = fw.sb("consA", [128, 1024], F32); Rc = Res()
    fw.dma("sp", cons[:], g.consts.ap, reads=[g.consts.R], writes=[Rc])
    pp = fw.sb("ppA", [128, NPP], F32); Rp = Res()
    fw.dma("act", pp[:], g.pp.ap[l], reads=[g.pp.R], writes=[Rp])
    return cons, Rc, pp, Rp


def ppc(pp, name, i=0, w=1):
    o, _ = PP[name]
    return pp[:, o + i:o + i + w]


def conv_seq(fw, g, row0, pad, Rpad, dst, Rdst, wcol, bcol, Rp):
    segs = [(0, 256, 0), (256, 4096, 260)]
    for (t0, L, po) in segs:
        fw.memset("pool", pad[:, po:po + 1], 0.0, pwrites=[Rpad])
        fw.memset("pool", pad[:, po + 1 + L:po + 3 + L], 0.0, pwrites=[Rpad])
        fw.dma("sp", pad[:, po + 1:po + 1 + L], g.projT.ap[row0:row0 + 128, t0:t0 + L], reads=[g.projT.R], pwrites=[Rpad])
    for (t0, L, po) in segs:
        fw.ts("dve", dst[:, t0:t0 + L], pad[:, po:po + L], wcol(0), bcol, ALU.mult, ALU.add, reads=[Rpad, Rp], pwrites=[Rdst])
        for j in range(1, 4):
            fw.stt("dve", dst[:, t0:t0 + L], pad[:, po + j:po + j + L], wcol(j), dst[:, t0:t0 + L], ALU.mult, ALU.add,
                   reads=[Rpad, Rp, Rdst], pwrites=[Rdst])


def rms_out(fw, g, src, Rsrc, nch, wcol, Rp, ones, Rc, row0, nfeat):
    sq = [fw.sb(f"rsq{i}", [128, nch, 512], F32) for i in range(2)]; Rsq = [Res(), Res()]
    rs = [fw.sb(f"rrs{i}", [128, 512], F32) for i in range(2)]; Rrs = [Res(), Res()]
    ob = [fw.sb(f"rob{i}", [128, nch, 512], BF16) for i in range(2)]; Rob = [Res(), Res()]
    pss = [fw.ps(f"rps{i}", [128, 512]) for i in range(2)]; Rps = [Res(), Res()]
    for bi, (t0, n) in enumerate(BLOCKS):
        b = bi % 2
        fw.act(sq[b][:, :, :n], src[:, :, t0:t0 + n], AF.Square, reads=[Rsrc], writes=[Rsq[b]])
        for c in range(nch):
            fw.mm(pss[b][:, :n], ones, sq[b][:, c, :n], c == 0, c == nch - 1, reads=[Rsq[b], Rc], pwrites=[Rps[b]])
        fw.act(rs[b][:, :n], pss[b][:, :n], AF.Ln, bias=EPS, scale=1.0 / nfeat, reads=[Rps[b]], writes=[Rrs[b]])
        fw.act(rs[b][:, :n], rs[b][:, :n], AF.Exp, scale=-0.5, reads=[Rrs[b]], writes=[Rrs[b]])
        for c in range(nch):
            fw.stt("dve", ob[b][:, c, :n], src[:, c, t0:t0 + n], wcol(c), rs[b][:, :n], ALU.mult, ALU.mult,
                   reads=[Rsrc, Rrs[b], Rp], pwrites=[Rob[b]])
            fw.dma("sp", g.mergedT.ap[row0 + c * 128:row0 + (c + 1) * 128, t0:t0 + n], ob[b][:, c, :n],
                   reads=[Rob[b]], pwrites=[g.mergedT.R])


def S2_rg(fw, g, l):
    with fw.stage():
        cons, Rc, pp, Rp = load_common(fw, g, l)
        ones = cons[:, C_ONES:C_ONES + 128]
        wg = fw.sb("rgw", [128, 8, 128], F32); Rwg = Res()
        fw.memset("pool", wg[:], 0.0, writes=[Rwg])
        for gate, W in enumerate((g.rg_wa, g.rg_wx)):
            for d in range(2):
                for cc in range(2):
                    for i in range(2):
                        fw.dma("sp", wg[i * 64:(i + 1) * 64, gate * 4 + d * 2 + cc, i * 64:(i + 1) * 64],
                               W.ap[l, d, 2 * cc + i], reads=[W.R], pwrites=[Rwg])
        spl = fw.sb("spl", [128, 4], F32); m8 = fw.sb("m8", [128, 4], F32); m16 = fw.sb("m16", [128, 4], F32); Rs = Res()
        fw.act(spl[:], ppc(pp, "rlam", 0, 4), AF.Exp, scale=-1.0, reads=[Rp], writes=[Rs])
        fw.act(spl[:], spl[:], AF.Ln, bias=1.0, scale=1.0, reads=[Rs], writes=[Rs])
        fw.ts("dve", m8[:], spl[:], -8.0, None, ALU.mult, reads=[Rs], pwrites=[Rs])
        fw.ts("dve", m16[:], spl[:], -16.0000001, None, ALU.mult, reads=[Rs], pwrites=[Rs])
        A = fw.sb("rgA", [128, 4360], F32); RA = Res()
        u = fw.sb("rgu", [128, NT], F32); Ru = Res()
        rt = fw.sb("rgr", [128, NT], F32); Rrt = Res()
        it = fw.sb("rgi", [128, NT], F32); Rit = Res()
        tmp = fw.sb("rgt", [128, NT], F32); Rtmp = Res()
        hs = fw.sb("rgh", [128, NT], F32); Rhs = Res()
        yg = fw.sb("rgy", [128, 2, NT], F32); Ryg = Res()
        pr = [fw.ps(f"rgp{i}", [128, 512]) for i in range(4)]; Rpr = [Res() for _ in range(4)]
        for cc in range(2):
            conv_seq(fw, g, 1544 + cc * 128, A, RA, u, Ru, lambda j: ppc(pp, "rcw", cc * 4 + j), ppc(pp, "rcb", cc), Rp)
            for d in range(2):
                k = 0
                for (t0, n) in BLOCKS:
                    for gate, (dst, Rd, bn) in enumerate(((rt, Rrt, "rba"), (it, Rit, "rbx"))):
                        b = k % 4; k += 1
                        fw.mm(pr[b][:, :n], wg[:, gate * 4 + d * 2 + cc, :], u[:, t0:t0 + n], True, True,
                              reads=[Rwg, Ru], pwrites=[Rpr[b]])
                        fw.act(dst[:, t0:t0 + n], pr[b][:, :n], AF.Sigmoid, bias=ppc(pp, bn, d * 2 + cc), scale=1.0,
                               reads=[Rpr[b], Rp], pwrites=[Rd])
                sc8 = m8[:, d * 2 + cc:d * 2 + cc + 1]; sc16 = m16[:, d * 2 + cc:d * 2 + cc + 1]
                fw.act(tmp[:], rt[:], AF.Exp, scale=sc16, reads=[Rrt, Rs], writes=[Rtmp])
                fw.ts("dve", tmp[:], tmp[:], -1.0, 1.0, ALU.mult, ALU.add, reads=[Rtmp], writes=[Rtmp])
                fw.ts("dve", tmp[:], tmp[:], 0.0, None, ALU.max, reads=[Rtmp], writes=[Rtmp])
                fw.act(tmp[:], tmp[:], AF.Sqrt, reads=[Rtmp], writes=[Rtmp])
                fw.act(rt[:], rt[:], AF.Exp, scale=sc8, reads=[Rrt, Rs], writes=[Rrt])
                fw.tt("dve", it[:], it[:], tmp[:], ALU.mult, reads=[Rit, Rtmp], writes=[Rit])
                fw.tt("dve", it[:], it[:], u[:], ALU.mult, reads=[Rit, Ru], writes=[Rit])
                out = hs if d == 0 else tmp
                Ro = Rhs if d == 0 else Rtmp
                if d == 0:
                    fw.op("dve", lambda e, out=out: e.tensor_tensor_scan(out=out[:, 0:256], data0=rt[:, 0:256], data1=it[:, 0:256],
                          initial=0.0, op0=ALU.mult, op1=ALU.add), reads=[Rrt, Rit], writes=[Ro])
                    fw.op("dve", lambda e, out=out: e.tensor_tensor_scan(out=out[:, 256:NT], data0=rt[:, 256:NT], data1=it[:, 256:NT],
                          initial=out[:, 255:256], op0=ALU.mult, op1=ALU.add), reads=[Rrt, Rit, Ro], pwrites=[Ro])
                else:
                    fw.op("dve", lambda e, out=out: e.tensor_tensor_scan(out=out[:, 0:256][:, ::-1], data0=rt[:, 0:256][:, ::-1],
                          data1=it[:, 0:256][:, ::-1], initial=0.0, op0=ALU.mult, op1=ALU.add), reads=[Rrt, Rit], writes=[Ro])
                    fw.op("dve", lambda e, out=out: e.tensor_tensor_scan(out=out[:, 256:NT][:, ::-1], data0=rt[:, 256:NT][:, ::-1],
                          data1=it[:, 256:NT][:, ::-1], initial=out[:, 0:1], op0=ALU.mult, op1=ALU.add), reads=[Rrt, Rit, Ro], pwrites=[Ro])
                    fw.tt("dve", hs[:], hs[:], tmp[:], ALU.add, reads=[Rhs, Rtmp], writes=[Rhs])
            gt = A[:, 0:NT]
            fw.dma("sp", gt, g.projT.ap[1800 + cc * 128:1800 + (cc + 1) * 128, :], reads=[g.projT.R], writes=[RA])
            fw.act(tmp[:], gt, AF.Square, reads=[RA], writes=[Rtmp])
            fw.ts("dve", tmp[:], tmp[:], 0.044715, 1.0, ALU.mult, ALU.add, reads=[Rtmp], writes=[Rtmp])
            fw.tt("dve", tmp[:], tmp[:], gt, ALU.mult, reads=[Rtmp, RA], writes=[Rtmp])
            fw.act(tmp[:], tmp[:], AF.Sigmoid, scale=1.5957691216057308, reads=[Rtmp], writes=[Rtmp])
            fw.tt("dve", tmp[:], tmp[:], gt, ALU.mult, reads=[Rtmp, RA], writes=[Rtmp])
            fw.tt("dve", yg[:, cc, :], hs[:], tmp[:], ALU.mult, reads=[Rhs, Rtmp], pwrites=[Ryg])
        rms_out(fw, g, yg, Ryg, 2, lambda c: ppc(pp, "rn", c), Rp, ones, Rc, 768, 256)


DBG = {}


def S3_ssd(fw, g, l):
    with fw.stage():
        cons, Rc, pp, Rp = load_common(fw, g, l)
        ident = cons[:, C_ID:C_ID + 128]
        ones = cons[:, C_ONES:C_ONES + 128]
        A = fw.sb("sA", [128, 4360], F32); RA = Res()
        xa = fw.sb("sxa", [128, 4, NT], F32); Rxa = [Res() for _ in range(4)]
        for c in range(4):
            conv_seq(fw, g, 768 + c * 128, A, RA, xa[:, c, :], Rxa[c], lambda j: ppc(pp, "scw", c * 4 + j), ppc(pp, "scb", c), Rp)
            fw.act(xa[:, c, :], xa[:, c, :], AF.Silu, reads=[Rxa[c]], writes=[Rxa[c]])
        dtt = fw.sb("sdt", [128, NTT, 8], F32); Rdt = Res()
        da = fw.sb("sda", [128, NTT, 8], F32); Rda = Res()
        bb = fw.sb("sbb", [128, 2, 8], F32); Rbb = Res()
        for tt in range(NTT):
            fw.dma("sp" if tt % 2 else "act", dtt[:, tt, :], g.dttok.ap[tt * 128:(tt + 1) * 128, :], reads=[g.dttok.R], pwrites=[Rdt])
        fw.dma("act", bb[:, 0, :], g.ssd_dt_bias.ap[l].partition_broadcast(128), reads=[g.ssd_dt_bias.R], pwrites=[Rbb])
        fw.dma("act", bb[:, 1, :], g.ssd_a_log.ap[l].partition_broadcast(128), reads=[g.ssd_a_log.R], pwrites=[Rbb])
        fw.act(bb[:, 1, :], bb[:, 1, :], AF.Exp, reads=[Rbb], pwrites=[Rbb])
        fw.ts("dve", bb[:, 1, :], bb[:, 1, :], -1.0, None, ALU.mult, reads=[Rbb], pwrites=[Rbb])
        for tt in range(NTT):
            fw.tt("dve", dtt[:, tt, :], dtt[:, tt, :], bb[:, 0, :], ALU.add, reads=[Rdt, Rbb], pwrites=[Rdt])
        dtf = dtt[:].rearrange("p t e -> p (t e)")
        fw.act(dtf, dtf, AF.Exp, reads=[Rdt], writes=[Rdt])
        fw.act(dtf, dtf, AF.Ln, bias=1.0, scale=1.0, reads=[Rdt], writes=[Rdt])
        for tt in range(NTT):
            fw.tt("dve", da[:, tt, :], dtt[:, tt, :], bb[:, 1, :], ALU.mult, reads=[Rdt, Rbb], pwrites=[Rda])
        yT = fw.sb("syT", [128, 2, NT], F32); RyT = Res()
        SinP = fw.sb("sSin", [128, 2, 128], F32); RS = [Res(), Res()]
        fw.memset("pool", SinP[:], 0.0, writes=RS)
        xtok = fw.sb("sxtok", [128, 384], F32); Rxtok = Res()
        rhsU = fw.sb("srhsU", [128, 4, 128], F32); RrhsU = Res()
        cscol = fw.sb("scscol", [128, 4], F32); Rcscol = Res()
        Ecs = fw.sb("sEcs", [128, 4, 128], F32); REcs = Res()
        csb = fw.sb("scsb", [128, 4, 128], F32); Rcsb = Res()
        dm = fw.sb("sdm", [128, 4, 128], F32); Rdm = Res()
        dcol = fw.sb("sdcol", [128, 4], F32); Rdcol = Res()
        xdt = fw.sb("sxdt", [128, 4, 64], F32); Rxdt = Res()
        xdd = fw.sb("sxdd", [128, 4, 64], F32); Rxdd = Res()
        Ce = fw.sb("sCe", [128, 4, 128], F32); RCe = Res()
        Mt = fw.sb("sMt", [128, 4, 128], F32); RMt = Res()
        Bm = fw.sb("sBm", [128, 2, 128], F32); RBm = Res()
        ptk = fw.ps("sptk", [128, 512]); Rptk = Res()
        pcs = fw.ps("spcs", [128, 4, 128]); Rpcs = Res()
        pcc = fw.ps("spcc", [128, 512]); Rpcc = Res()
        Gsb = fw.sb("sGsb", [128, 2, 128], F32); RGsb = Res()
        ysb = fw.sb("sysb", [128, 4, 128], F32); Rysb = Res()
        Ssb = fw.sb("sSsb", [128, 2, 128], F32); RSsb = Res()
        pG = fw.ps("spG", [128, 4, 128]); RpG = Res()
        py = fw.ps("spy", [128, 4, 128]); Rpy = Res()
        pS = fw.ps("spS", [128, 4, 128]); RpS = Res()
        for d in range(2):
            order = list(range(NTT)) if d == 0 else [1, 0] + list(range(NTT - 1, 1, -1))
            Um = cons[:, C_U:C_U + 128] if d == 0 else cons[:, C_UT:C_UT + 128]
            nm = cons[:, C_NMF:C_NMF + 128] if d == 0 else cons[:, C_NMB:C_NMB + 128]
            ec = 127 if d == 0 else 0
            for tt in order[:DBG.get('units', 99)]:
                sl = slice(tt * 128, (tt + 1) * 128)
                for c in range(3):
                    fw.tr(ptk[:, c * 128:(c + 1) * 128], xa[:, c, sl], ident, reads=[Rxa[c], Rc], pwrites=[Rptk])
                fw.copy("act", xtok[:], ptk[:, 0:384], reads=[Rptk], writes=[Rxtok])
                if DBG.get('step', 99) < 2:
                    continue
                for h in range(4):
                    fw.ts("dve", rhsU[:, h, :], Um, da[:, tt, d * 4 + h:d * 4 + h + 1], None, ALU.mult, reads=[Rc, Rda], pwrites=[RrhsU])
                fw.mm(pcs[:].rearrange("p h l -> p (h l)"), ones, rhsU[:].rearrange("p h l -> p (h l)"), True, True,
                      reads=[Rc, RrhsU], pwrites=[Rpcs])
                fw.mm(pcc[:, 0:4], Um, da[:, tt, d * 4:d * 4 + 4], True, True, reads=[Rc, Rda], pwrites=[Rpcc])
                fw.copy("act", cscol[:], pcc[:, 0:4], reads=[Rpcc], writes=[Rcscol])
                if DBG.get('step', 99) < 3:
                    continue
                fw.copy("dve", csb[:].rearrange("p h l -> p (h l)"), pcs[:].rearrange("p h l -> p (h l)"), reads=[Rpcs], writes=[Rcsb])
                fw.act(Ecs[:].rearrange("p h l -> p (h l)"), csb[:].rearrange("p h l -> p (h l)"), AF.Exp, reads=[Rcsb], writes=[REcs])
                for h in range(4):
                    fw.stt("dve", dm[:, h, :], csb[:, h, :], cscol[:, h:h + 1], nm, ALU.subtract, ALU.min,
                           reads=[Rcsb, Rcscol, Rc], pwrites=[Rdm])
                fw.act(dm[:].rearrange("p h l -> p (h l)"), dm[:].rearrange("p h l -> p (h l)"), AF.Exp, reads=[Rdm], writes=[Rdm])
                fw.tt("dve", dcol[:], csb[:, :, ec], cscol[:], ALU.subtract, reads=[Rcsb, Rcscol], writes=[Rdcol])
                fw.act(dcol[:], dcol[:], AF.Exp, reads=[Rdcol], writes=[Rdcol])
                if DBG.get('step', 99) < 4:
                    continue
                for h in range(4):
                    fw.ts("dve", xdt[:, h, :], xtok[:, h * 64:(h + 1) * 64], dtt[:, tt, d * 4 + h:d * 4 + h + 1], None, ALU.mult,
                          reads=[Rxtok, Rdt], pwrites=[Rxdt])
                for h in range(4):
                    fw.ts("dve", xdd[:, h, :], xdt[:, h, :], dcol[:, h:h + 1], None, ALU.mult, reads=[Rxdt, Rdcol], pwrites=[Rxdd])
                if DBG.get('step', 99) < 5:
                    continue
                for h in range(4):
                    mcol = cons[:, C_BONES + 64 * (h // 2):C_BONES + 64 * (h // 2) + 1]
                    fw.stt("dve", Ce[:, h, :], xa[:, 3, sl], mcol, Ecs[:, h, :], ALU.mult, ALU.mult, reads=[Rxa[3], REcs, Rc], pwrites=[RCe])
                for gg in range(2):
                    mcol = cons[:, C_BONES + 64 * gg:C_BONES + 64 * gg + 1]
                    fw.ts("dve", Bm[:, gg, :], xa[:, 2, sl], mcol, None, ALU.mult, reads=[Rxa[2], Rc], pwrites=[RBm])
                for gg in range(2):
                    fw.mm(pG[:, gg, :], Bm[:, gg, :], xa[:, 3, sl], True, True, reads=[RBm, Rxa[3]], pwrites=[RpG])
                fw.copy("act", Gsb[:].rearrange("p a b -> p (a b)"), pG[:, 0:2, :].rearrange("p a b -> p (a b)"), reads=[RpG], writes=[RGsb])
                for h in range(4):
                    fw.tt("dve", Mt[:, h, :], Gsb[:, h // 2, :], dm[:, h, :], ALU.mult, reads=[RGsb, Rdm], pwrites=[RMt])
                if DBG.get('step', 99) < 6:
                    continue
                for h in range(4):
                    j = h // 2
                    ps_ = slice(j * 64, (j + 1) * 64)
                    fw.mm(py[:, h, :], xdt[:, 2 * j:2 * j + 2, :].rearrange("p a b -> p (a b)"), Mt[:, h, :], True, False,
                          reads=[Rxdt, RMt], pwrites=[Rpy])
                    fw.mm(py[:, h, :], SinP[:, d, :], Ce[:, h, :], False, True, reads=[RS[d], RCe], pwrites=[Rpy])
                if DBG.get('step', 99) < 7:
                    continue
                for h in range(4):
                    j, i = h // 2, h % 2
                    rows = slice(i * 64, (i + 1) * 64)
                    if h == 0:
                        fw.copy("act", ysb[:].rearrange("p a b -> p (a b)"), py[:].rearrange("p a b -> p (a b)"), reads=[Rpy], writes=[Rysb])
                    if d == 0:
                        fw.copy("dve", yT[rows, j, sl], ysb[rows, h, :], reads=[Rysb], pwrites=[RyT])
                    else:
                        fw.tt("dve", yT[rows, j, sl], ysb[rows, h, :], yT[rows, j, sl], ALU.add, reads=[Rysb, RyT], pwrites=[RyT])
                if DBG.get('step', 99) < 8:
                    continue
                for j in range(2):
                    fw.mm(pS[:, j, :], xtok[:, 256:384], xdd[:, 2 * j:2 * j + 2, :].rearrange("p a b -> p (a b)"), True, True,
                          reads=[Rxtok, Rxdd], pwrites=[RpS])
                fw.copy("act", Ssb[:].rearrange("p a b -> p (a b)"), pS[:, 0:2, :].rearrange("p a b -> p (a b)"), reads=[RpS], writes=[RSsb])
                for h in range(4):
                    j, i = h // 2, h % 2
                    rows = slice(j * 64, (j + 1) * 64)
                    cols = slice(i * 64, (i + 1) * 64)
                    fw.stt("dve", SinP[rows, d, cols], SinP[rows, d, cols], Ecs[rows, h, ec:ec + 1], Ssb[rows, j, cols], ALU.mult, ALU.add,
                           reads=[RS[d], REcs, RSsb], pwrites=[RS[d]])
        for j in range(2):
            fw.stt("dve", yT[:, j, :], xa[:, j, :], ppc(pp, "sd", j), yT[:, j, :], ALU.mult, ALU.add, reads=[Rxa[j], Rp, RyT], pwrites=[RyT])
            zt = A[:, 0:NT]
            fw.dma("sp", zt, g.projT.ap[1280 + j * 128:1280 + (j + 1) * 128, :], reads=[g.projT.R], writes=[RA])
            fw.act(zt, zt, AF.Silu, reads=[RA], writes=[RA])
            fw.tt("dve", yT[:, j, :], yT[:, j, :], zt, ALU.mult, reads=[RyT, RA], pwrites=[RyT])
        rms_out(fw, g, yT, RyT, 2, lambda c: ppc(pp, "sn", c), Rp, ones, Rc, 512, 256)


class LNCtx:
    def __init__(self, fw, g, l, wname, bname):
        self.w = fw.sb("lnw", [128, 1024], F32); self.b = fw.sb("lnb", [128, 1024], F32); self.R = Res()
        W = getattr(g, wname); B = getattr(g, bname)
        fw.dma("sp", self.w[:], W.ap[l].partition_broadcast(128), reads=[W.R], pwrites=[self.R])
        fw.dma("act", self.b[:], B.ap[l].partition_broadcast(128), reads=[B.R], pwrites=[self.R])
        self.sq = fw.sb("lnsq", [128, 1024], F32); self.Rsq = Res()
        self.st = fw.sb("lnst", [128, 8], F32); self.Rst = Res()
        self.o = [fw.sb(f"lno{i}", [128, 1024], F32) for i in range(2)]; self.Ro = [Res(), Res()]
        self.k = 0


def ln_tile(fw, L, y, Ry, dst_ap, dstR, is_output=False):
    st = L.st
    fw.op("dve", lambda e: e.tensor_reduce(out=st[:, 0:1], in_=y, axis=AX.X, op=ALU.add), reads=[Ry], pwrites=[L.Rst])
    fw.act(L.sq[:], y, AF.Square, reads=[Ry], writes=[L.Rsq])
    fw.op("dve", lambda e: e.tensor_reduce(out=st[:, 1:2], in_=L.sq[:], axis=AX.X, op=ALU.add), reads=[L.Rsq], pwrites=[L.Rst])
    fw.ts("dve", st[:, 2:3], st[:, 0:1], 1.0 / 1024, None, ALU.mult, reads=[L.Rst], pwrites=[L.Rst])
    fw.tt("dve", st[:, 3:4], st[:, 2:3], st[:, 2:3], ALU.mult, reads=[L.Rst], pwrites=[L.Rst])
    fw.stt("dve", st[:, 4:5], st[:, 1:2], 1.0 / 1024, st[:, 3:4], ALU.mult, ALU.subtract, reads=[L.Rst], pwrites=[L.Rst])
    fw.act(st[:, 5:6], st[:, 4:5], AF.Ln, bias=EPS, scale=1.0, reads=[L.Rst], pwrites=[L.Rst])
    fw.act(st[:, 5:6], st[:, 5:6], AF.Exp, scale=-0.5, reads=[L.Rst], pwrites=[L.Rst])
    fw.stt("dve", st[:, 6:7], st[:, 2:3], -1.0, st[:, 5:6], ALU.mult, ALU.mult, reads=[L.Rst], pwrites=[L.Rst])
    b = L.k % 2; L.k += 1
    o = L.o[b]
    fw.act(o[:], y, AF.Identity, bias=st[:, 6:7], scale=st[:, 5:6], reads=[Ry, L.Rst], writes=[L.Ro[b]])
    fw.tt("dve", o[:], o[:], L.w[:], ALU.mult, reads=[L.Ro[b], L.R], writes=[L.Ro[b]])
    fw.tt("dve", o[:], o[:], L.b[:], ALU.add, reads=[L.Ro[b], L.R], writes=[L.Ro[b]])
    fw.dma("sp", dst_ap, o[:], reads=[L.Ro[b]], pwrites=[dstR], is_output=is_output)


def S4_attn(fw, g, l, xsrc, csrc, xdst, cdst, do_ctx):
    with fw.stage():
        cons, Rc, pp, Rp = load_common(fw, g, l)
        ident = cons[:, C_ID:C_ID + 128]; ones = cons[:, C_ONES:C_ONES + 128]
        bones = cons[:, C_BONES:C_BONES + 128]; Rtm = cons[:, C_RT:C_RT + 128]
        g1b = fw.sb("g1b", [128, 2, 1024], F32); Rg1 = Res()
        for r in range(2):
            fw.dma("sp", g1b[:, r, :], g.modv.ap[l, r, 2048:3072].partition_broadcast(128), reads=[g.modv.R], pwrites=[Rg1])
        L = LNCtx(fw, g, l, "ln1_w", "ln1_b")
        woa = fw.sb("woa", [64, 8, 1024], BF16); wob = fw.sb("wob", [128, 4, 1024], BF16); Rwo = Res()
        for h in range(8):
            fw.dma("pool", woa[:, h, :], g.w_out.ap[l, h * 64:(h + 1) * 64, :], reads=[g.w_out.R], pwrites=[Rwo])
        for c in range(4):
            fw.dma("pool", wob[:, c, :], g.w_out.ap[l, 512 + c * 128:512 + (c + 1) * 128, :], reads=[g.w_out.R], pwrites=[Rwo])
        Vx = fw.sb("Vx", [128, NTT, 2, 128], BF16); RVx = Res()
        fw.memset("pool", Vx[:].rearrange("p t g d -> p (t g d)"), 1.0, writes=[RVx])
        for tt in range(NTT):
            fw.dma("pool", Vx[:, tt, :, 0:64], g.vtok.ap[tt * 128:(tt + 1) * 128, :].rearrange("p (g d) -> p g d", g=2),
                   reads=[g.vtok.R], pwrites=[RVx])
        kTm = fw.sb("kTm", [128, 2, NT], BF16); RkT = Res()
        raw = [fw.sb(f"raw{i}", [128, 512], F32) for i in range(2)]; Rraw = [Res(), Res()]
        sq = fw.sb("qsq", [128, 512], F32); Rsq = Res()
        rs = fw.sb("qrs", [128, 512], F32); Rrs = Res()
        kn = fw.sb("qkn", [128, 512], F32); Rkn = Res()
        t1 = fw.sb("qt1", [128, 512], F32); Rt1 = Res()
        t2 = fw.sb("qt2", [128, 512], F32); Rt2 = Res()
        rope = fw.sb("ropeb", [128, 2, 512], F32); Rrope = Res()
        pprep = [fw.ps(f"pprep{i}", [128, 512]) for i in range(2)]; Rpp_ = [Res(), Res()]
        cnt = [0]

        def qk_block(n, wcol, rope_t0, outs):
            b = cnt[0] % 2
            r_ = raw[b]; Rr = Rraw[b]
            fw.act(sq[:, :n], r_[:, :n], AF.Square, reads=[Rr], writes=[Rsq])
            fw.mm(pprep[0][:, :n], bones, sq[:, :n], True, True, reads=[Rc, Rsq], pwrites=[Rpp_[0]])
            fw.act(rs[:, :n], pprep[0][:, :n], AF.Ln, bias=EPS, scale=1.0 / 64, reads=[Rpp_[0]], writes=[Rrs])
            fw.act(rs[:, :n], rs[:, :n], AF.Exp, scale=-0.5, reads=[Rrs], writes=[Rrs])
            fw.stt("dve", kn[:, :n], r_[:, :n], wcol, rs[:, :n], ALU.mult, ALU.mult, reads=[Rr, Rp, Rrs], writes=[Rkn])
            src, Rsrc = kn, Rkn
            if rope_t0 is not None:
                fw.dma("sp", rope[:, 0, :n], g.rope.ap[:, rope_t0:rope_t0 + n], reads=[g.rope.R], pwrites=[Rrope])
                fw.dma("act", rope[:, 1, :n], g.rope.ap[:, 4096 + rope_t0:4096 + rope_t0 + n], reads=[g.rope.R], pwrites=[Rrope])
                fw.mm(pprep[1][:, :n], Rtm, kn[:, :n], True, True, reads=[Rc, Rkn], pwrites=[Rpp_[1]])
                fw.tt("dve", t2[:, :n], pprep[1][:, :n], rope[:, 1, :n], ALU.mult, reads=[Rpp_[1], Rrope], writes=[Rt2])
                fw.tt("dve", t1[:, :n], kn[:, :n], rope[:, 0, :n], ALU.mult, reads=[Rkn, Rrope], writes=[Rt1])
                fw.tt("dve", t1[:, :n], t1[:, :n], t2[:, :n], ALU.add, reads=[Rt1, Rt2], writes=[Rt1])
                src, Rsrc = t1, Rt1
            for (oap, mcol, Ro) in outs:
                if mcol is None:
                    fw.copy("dve", oap, src[:, :n], reads=[Rsrc], pwrites=[Ro])
                else:
                    fw.ts("dve", oap, src[:, :n], mcol, None, ALU.mult, reads=[Rsrc, Rc], pwrites=[Ro])
            cnt[0] += 1

        for (t0, n) in BLOCKS:
            b = cnt[0] % 2
            fw.dma("sp", raw[b][:, :n], g.projT.ap[512:640, t0:t0 + n], reads=[g.projT.R], writes=[Rraw[b]])
            qk_block(n, ppc(pp, "kw"), None if t0 < 256 else t0 - 256,
                     [(kTm[:, gg, t0:t0 + n], cons[:, C_BONES + 64 * gg:C_BONES + 64 * gg + 1], RkT) for gg in range(2)])
        qT = fw.sb("qT", [128, 4, 512], BF16); RqT = Res()
        pT = [fw.sb(f"pT{i}", [128, 512], BF16) for i in range(4)]; RpT = [Res() for _ in range(4)]
        osb = fw.sb("osb", [128, 512], F32); Rosb = Res()
        denb = fw.sb("denb", [64, 512], F32); Rdenb = Res()
        attnb = fw.sb("attnb", [64, 8, 512], F32); Rattnb = Res()
        sqb = fw.sb("sqb", [64, 8, 512], F32); Rsqb = Res()
        attn_n = fw.sb("attn_n", [64, 8, 512], BF16); Rattn_n = Res()
        sr = fw.sb("sr", [128, 4, 512], BF16); Rsr = Res()
        xt = [fw.sb(f"xt{i}", [128, 1024], F32) for i in range(2)]; Rxt = [Res(), Res()]
        ysb = [fw.sb(f"ysb{i}", [128, 1024], F32) for i in range(2)]; Rysb = [Res(), Res()]
        psc = [fw.ps(f"psc{i}", [128, 512]) for i in range(3)]; Rpsc = [Res(), Res(), Res()]
        po = [fw.ps(f"po{i}", [128, 512]) for i in range(2)]; Rpo = [Res(), Res()]
        pso = [fw.ps("pso0", [128, 512])] * 2; Rpso = [Res()] * 2
        blocks = [(256 + i * 512, 512, list(range(NTT)), 0) for i in range(8)]
        if do_ctx:
            blocks.append((0, 256, [0, 1], 1))
        kk = 0
        hk = 0
        tk = 0
        for (t0, n, ktiles, r) in blocks:
            for jp in range(4):
                b = cnt[0] % 2
                fw.dma("sp", raw[b][0:64, :n], g.projT.ap[jp * 64:(jp + 1) * 64, t0:t0 + n], reads=[g.projT.R], pwrites=[Rraw[b]])
                fw.dma("act", raw[b][64:128, :n], g.projT.ap[(jp + 4) * 64:(jp + 5) * 64, t0:t0 + n], reads=[g.projT.R], pwrites=[Rraw[b]])
                qk_block(n, ppc(pp, "qw"), None if r == 1 else t0 - 256, [(qT[:, jp, :n], None, RqT)])
            for c in range(4):
                fw.dma("act", sr[:, c, :n], g.mergedT.ap[512 + c * 128:512 + (c + 1) * 128, t0:t0 + n], reads=[g.mergedT.R], pwrites=[Rsr])
            its = []
            for jp in range(4):
                for half in range(2):
                    h = jp + 4 * half
                    pob = hk % 2; hk += 1
                    for ki, kc in enumerate(ktiles):
                        its.append((jp, half, h, pob, ki, kc, kk % 3, kk % 4))
                        kk += 1

            def emit_pv(it):
                jp, half, h, pob, ki, kc, sb_, pb = it
                fw.mm(po[pob][:, :n], Vx[:, kc, half, :], pT[pb][:, :n], ki == 0, ki == len(ktiles) - 1,
                      reads=[RVx, RpT[pb]], pwrites=[Rpo[pob]])
                if ki == len(ktiles) - 1:
                    fw.copy("act", osb[:, :n], po[pob][:, :n], reads=[Rpo[pob]], writes=[Rosb])
                    fw.copy("dve", denb[:, :n], osb[64:128, :n], reads=[Rosb], writes=[Rdenb])
                    fw.op("dve", lambda e, n=n: e.reciprocal(out=denb[:, :n], in_=denb[:, :n]), reads=[Rdenb], writes=[Rdenb])
                    fw.tt("dve", attnb[:, h, :n], osb[0:64, :n], denb[:, :n], ALU.mult, reads=[Rosb, Rdenb], pwrites=[Rattnb])

            pend = []
            for it in its:
                jp, half, h, pob, ki, kc, sb_, pb = it
                fw.mm(psc[sb_][:, :n], kTm[:, half, kc * 128:(kc + 1) * 128], qT[:, jp, :n], True, True,
                      reads=[RkT, RqT], pwrites=[Rpsc[sb_]])
                fw.act(pT[pb][:, :n], psc[sb_][:, :n], AF.Exp, scale=0.125, reads=[Rpsc[sb_]], writes=[RpT[pb]])
                pend.append(it)
                if len(pend) > 2:
                    emit_pv(pend.pop(0))
            for it in pend:
                emit_pv(it)
            for h in range(8):
                fw.act(sqb[:, h, :n], attnb[:, h, :n], AF.Square, reads=[Rattnb], pwrites=[Rsqb])
            for h in range(8):
                fw.mm(pprep[0][:, :n], cons[0:64, C_ONES:C_ONES + 128], sqb[:, h, :n], h == 0, h == 7, reads=[Rc, Rsqb], pwrites=[Rpp_[0]])
            fw.act(rs[:, :n], pprep[0][:, :n], AF.Ln, bias=EPS, scale=1.0 / 512, reads=[Rpp_[0]], writes=[Rrs])
            fw.act(rs[:, :n], rs[:, :n], AF.Exp, scale=-0.5, reads=[Rrs], writes=[Rrs])
            for h in range(8):
                fw.stt("dve", attn_n[:, h, :n], attnb[:, h, :n], ppc(pp, "aon", h)[0:64, :], rs[0:64, :n], ALU.mult, ALU.mult,
                       reads=[Rattnb, Rp, Rrs], pwrites=[Rattn_n])
            for i in range(n // 128):
                tb = tk % 2; tk += 1
                tsl = slice(i * 128, (i + 1) * 128)
                if r == 0:
                    src_ap, sR = xsrc.ap[t0 - 256 + i * 128:t0 - 256 + (i + 1) * 128, :], xsrc.R
                    dst_ap, dR = xdst.ap[t0 - 256 + i * 128:t0 - 256 + (i + 1) * 128, :], xdst.R
                else:
                    src_ap, sR = csrc.ap[i * 128:(i + 1) * 128, :], csrc.R
                    dst_ap, dR = cdst.ap[i * 128:(i + 1) * 128, :], cdst.R
                fw.dma("act", xt[tb][:], src_ap, reads=[sR], writes=[Rxt[tb]])
                for hf in range(2):
                    cs_ = slice(hf * 512, (hf + 1) * 512)
                    for h in range(8):
                        fw.mm(pso[hf][:], attn_n[:, h, tsl], woa[:, h, cs_], h == 0, False, reads=[Rattn_n, Rwo], pwrites=[Rpso[hf]])
                    for c in range(4):
                        fw.mm(pso[hf][:], sr[:, c, tsl], wob[:, c, cs_], False, c == 3, reads=[Rsr, Rwo], pwrites=[Rpso[hf]])
                    fw.tt("dve", ysb[tb][:, cs_], pso[hf][:], g1b[:, r, cs_], ALU.mult, reads=[Rpso[hf], Rg1], pwrites=[Rysb[tb]])
                fw.stt("dve", ysb[tb][:], xt[tb][:], ALPHA, ysb[tb][:], ALU.mult, ALU.add, reads=[Rxt[tb], Rysb[tb]], writes=[Rysb[tb]])
                ln_tile(fw, L, ysb[tb][:], Rysb[tb], dst_ap, dR)


def S5_moe(fw, g, l, xsrc, csrc, xdst, cdst, do_ctx, final=False):
    sets = [dict(N=4096, cap=512, src=xsrc, h2=g.h2x, acc=g.accx, dst=xdst, r=0, s0=0)]
    if do_ctx:
        sets.append(dict(N=256, cap=32, src=csrc, h2=g.h2c, acc=g.accc, dst=cdst, r=1, s0=512))
    NS = 544 if do_ctx else 512
    with fw.stage():
        cons, Rc, pp, Rp = load_common(fw, g, l)
        ident = cons[:, C_ID:C_ID + 128]
        idb = fw.sb("idb", [128, 128], BF16); Ridb = Res()
        fw.dma("pool", idb[:], g.consts.ap[:, C_ID:C_ID + 128], reads=[g.consts.R], writes=[Ridb])
        modb = fw.sb("modb", [128, 2, 3, 1024], F32); Rmodb = Res()
        for r in range(2 if do_ctx else 1):
            for k, which in enumerate((4, 3, 5)):
                fw.dma("sp", modb[:, r, k, :], g.modv.ap[l, r, which * 1024:(which + 1) * 1024].partition_broadcast(128),
                       reads=[g.modv.R], pwrites=[Rmodb])
            fw.ts("dve", modb[:, r, 0, :], modb[:, r, 0, :], 1.0, None, ALU.add, reads=[Rmodb], pwrites=[Rmodb])
        for s in sets:
            nsc = max(1, s["cap"] // 128)
            s["nsc"] = nsc
            s["ns"] = min(128, s["cap"])
            s["idxT"] = fw.sb("idxT", [128, nsc, 16], I32); s["valsT"] = fw.sb("valsT", [128, nsc, 16], F32); s["Rsel"] = Res()
        with fw.stage():
            wr = fw.sb("wr", [128, 8, 16], F32); Rwr = Res()
            for j in range(8):
                fw.dma("sp", wr[:, j, :], g.w_router.ap[l, j * 128:(j + 1) * 128, :], reads=[g.w_router.R], pwrites=[Rwr])
            xt = [fw.sb(f"axt{i}", [128, 1024], F32) for i in range(2)]; Rxt = [Res(), Res()]
            hb = [fw.sb(f"ahb{i}", [128, 1024], BF16) for i in range(2)]; Rhb = [Res(), Res()]
            hT = [fw.sb(f"ahT{i}", [128, 8, 128], F32) for i in range(2)]; RhT = [Res(), Res()]
            zer = fw.sb("azer", [128, 1024], F32); Rzer = Res()
            fw.memset("pool", zer[:], 0.0, writes=[Rzer])
            ptr = [fw.ps(f"aptr{i}", [128, 512]) for i in range(4)]; Rptr = [Res() for _ in range(4)]
            plg = [fw.ps(f"aplg{i}", [128, 512]) for i in range(2)]; Rplg = [Res(), Res()]
            pden = fw.ps("apden", [128, 512]); Rpden = Res()
            ptx = fw.ps("aptx", [128, 512]); Rptx = Res()
            for s in sets:
                N, cap, r = s["N"], s["cap"], s["r"]
                aff = fw.sb("aff", [16, N], F32); Raff = Res()
                work = fw.sb("awork", [16, N], F32); Rwork = Res()
                vals = fw.sb("avals", [16, cap], F32); Rvals = Res()
                idxu = fw.sb("aidxu", [16, cap], U32); Ridxu = Res()
                idxf = fw.sb("aidxf", [16, cap], F32); Ridxf = Res()
                den = fw.sb("aden", [16, 512], F32); Rden = Res()
                for tt in range(N // 128):
                    b = tt % 2
                    rows = slice(tt * 128, (tt + 1) * 128)
                    fw.dma("sp", xt[b][:], s["src"].ap[rows, :], reads=[s["src"].R], writes=[Rxt[b]])
                    fw.dma("act", s["acc"].ap[rows, :], zer[:], reads=[Rzer], pwrites=[s["acc"].R])
                    fw.tt("dve", xt[b][:], xt[b][:], modb[:, r, 0, :], ALU.mult, reads=[Rxt[b], Rmodb], writes=[Rxt[b]])
                    fw.tt("dve", xt[b][:], xt[b][:], modb[:, r, 1, :], ALU.add, reads=[Rxt[b], Rmodb], writes=[Rxt[b]])
                    fw.copy("act", hb[b][:], xt[b][:], reads=[Rxt[b]], writes=[Rhb[b]])
                    fw.dma("sp", s["h2"].ap[rows, :], hb[b][:], reads=[Rhb[b]], pwrites=[s["h2"].R])
                    for half in range(2):
                        pi = b * 2 + half
                        for jj in range(4):
                            j = half * 4 + jj
                            fw.tr(ptr[pi][:, jj * 128:(jj + 1) * 128], xt[b][:, j * 128:(j + 1) * 128], ident, reads=[Rxt[b], Rc], pwrites=[Rptr[pi]])
                        fw.copy("act", hT[b][:, half * 4:(half + 1) * 4, :].rearrange("p a b -> p (a b)"), ptr[pi][:], reads=[Rptr[pi]], pwrites=[RhT[b]])
                    for j in range(8):
                        fw.mm(plg[b][0:16, 0:128], wr[:, j, :], hT[b][:, j, :], j == 0, j == 7, reads=[Rwr, RhT[b]], pwrites=[Rplg[b]])
                    fw.act(aff[:, rows], plg[b][0:16, 0:128], AF.Exp, reads=[Rplg[b]], pwrites=[Raff])
                for c0 in range(0, N, 512):
                    n = min(512, N - c0)
                    fw.mm(pden[0:16, :n], cons[0:16, C_ONES:C_ONES + 16], aff[:, c0:c0 + n], True, True, reads=[Rc, Raff], pwrites=[Rpden])
                    fw.op("dve", lambda e, n=n, den=den: e.reciprocal(out=den[:, :n], in_=pden[0:16, :n]), reads=[Rpden], writes=[Rden])
                    fw.tt("dve", aff[:, c0:c0 + n], aff[:, c0:c0 + n], den[:, :n], ALU.mult, reads=[Raff, Rden], pwrites=[Raff])
                fw.copy("dve", work[:], aff[:], reads=[Raff], writes=[Rwork])
                for it in range(cap // 8):
                    sl8 = slice(it * 8, (it + 1) * 8)
                    fw.op("dve", lambda e, sl8=sl8, vals=vals, work=work: e.max(out=vals[:, sl8], in_=work[:]), reads=[Rwork], pwrites=[Rvals])
                    fw.op("dve", lambda e, sl8=sl8, vals=vals, work=work, idxu=idxu: e.max_index(out=idxu[:, sl8], in_max=vals[:, sl8], in_values=work[:]),
                          reads=[Rwork, Rvals], pwrites=[Ridxu])
                    fw.op("dve", lambda e, sl8=sl8, vals=vals, work=work: e.match_replace(out=work[:], in_to_replace=vals[:, sl8], in_values=work[:], imm_value=-1.0),
                          reads=[Rvals, Rwork], writes=[Rwork])
                fw.copy("dve", idxf[:], idxu[:], reads=[Ridxu], writes=[Ridxf])
                ns = s["ns"]
                for sc in range(s["nsc"]):
                    fw.tr(ptx[0:ns, 0:16], idxf[:, sc * 128:sc * 128 + ns], cons[0:16, C_ID:C_ID + 16], reads=[Ridxf, Rc], pwrites=[Rptx])
                    fw.tr(ptx[0:ns, 16:32], vals[:, sc * 128:sc * 128 + ns], cons[0:16, C_ID:C_ID + 16], reads=[Rvals, Rc], pwrites=[Rptx])
                    tmp = fw.sb("atmp", [128, 32], F32); Rtmp = Res()
                    fw.copy("act", tmp[0:ns, :], ptx[0:ns, 0:32], reads=[Rptx], writes=[Rtmp])
                    fw.copy("dve", s["idxT"][0:ns, sc, :], tmp[0:ns, 0:16], reads=[Rtmp], pwrites=[s["Rsel"]])
                    fw.copy("dve", s["valsT"][0:ns, sc, :], tmp[0:ns, 16:32], reads=[Rtmp], pwrites=[s["Rsel"]])
        with fw.stage():
            wgu = [fw.sb(f"wgu{i}", [128, 2, 8, 1024], BF16) for i in range(2)]; Rwgu = [Res(), Res()]
            wd = [fw.sb(f"wd{i}", [128, 8, 1024], BF16) for i in range(2)]; Rwd = [Res(), Res()]
            xs = [fw.sb(f"xs{i}", [128, 1024], BF16) for i in range(2)]; Rxs = [Res(), Res()]
            xsT = fw.sb("xsT", [128, 8, NS], BF16); RxsT = Res()
            gsb = [fw.sb(f"gsb{i}", [128, 512], F32) for i in range(2)]; Rgsb = [Res(), Res()]
            gsc = fw.sb("gsc", [128, 64], F32); Rgsc = Res()
            hidT = fw.sb("hidT", [128, 8, NS], BF16); RhidT = Res()
            ntl = 5 if do_ctx else 4
            yacc = fw.sb("yacc", [128, ntl, 1024], F32); Ryacc = [Res() for _ in range(ntl)]
            ysc = [fw.sb(f"ysc{i}", [128, 1024], F32) for i in range(2)]; Rysc = [Res(), Res()]
            ptb = fw.ps("bptb", [128, 1024], BF16); Rptb = Res()
            pg = [fw.ps(f"bpg{i}", [128, 512]) for i in range(2)]; Rpg = [Res(), Res()]
            pu = [fw.ps(f"bpu{i}", [128, 512]) for i in range(2)]; Rpu = [Res(), Res()]
            pc = fw.ps("bpc", [128, 512]); Rpc = Res()
            pyy = [fw.ps(f"bpy{i}", [128, 512]) for i in range(2)]; Rpyy = [Res(), Res()]
            tiles = []
            for s in sets:
                for sc in range(s["nsc"]):
                    tiles.append((s, sc, s["ns"], s["s0"] + sc * 128))
            deferred = []
            xk = 0
            gk = 0
            yk = 0
            for e in range(16):
                for (s, sc, ns, sl0) in tiles:
                    b = xk % 2; xk += 1
                    fw.dma_custom("pool", lambda en, s=s, sc=sc, ns=ns, b=b, e=e: en.indirect_dma_start(
                        out=xs[b][0:ns, :], out_offset=None, in_=s["h2"].ap,
                        in_offset=bass.IndirectOffsetOnAxis(ap=s["idxT"][0:ns, sc, e:e + 1], axis=0)),
                        reads=[s["h2"].R, s["Rsel"]], writes=[Rxs[b]])
                    for j in range(8):
                        fw.tr(ptb[:, j * 128:j * 128 + ns], xs[b][0:ns, j * 128:(j + 1) * 128], idb[0:ns, 0:ns], reads=[Rxs[b], Ridb], pwrites=[Rptb])
                    fw.copy("act", xsT[:, :, sl0:sl0 + ns], ptb[:].rearrange("p (j s) -> p j s", j=8)[:, :, 0:ns], reads=[Rptb], pwrites=[RxsT])
                for fh in range(2):
                    wb = fh
                    for j in range(8):
                        rows = slice(j * 128, (j + 1) * 128)
                        cols = slice(fh * 1024, (fh + 1) * 1024)
                        wga, wgR = g.moe_w("gate", l, e)
                        wua, wuR = g.moe_w("up", l, e)
                        wda, wdR = g.moe_w("down", l, e)
                        fw.dma("pool", wgu[wb][:, 0, j, :], wga[rows, cols], reads=[wgR], pwrites=[Rwgu[wb]])
                        fw.dma("pool", wgu[wb][:, 1, j, :], wua[rows, cols], reads=[wuR], pwrites=[Rwgu[wb]])
                        fw.dma("pool", wd[wb][:, j, :], wda[fh * 1024 + j * 128:fh * 1024 + (j + 1) * 128, :], reads=[wdR], pwrites=[Rwd[wb]])
                    if fh == 0:
                        for f in deferred:
                            f()
                        deferred = []
                    for fc in range(8):
                        b = gk % 2; gk += 1
                        fcs = slice(fc * 128, (fc + 1) * 128)
                        for j in range(8):
                            fw.mm(pg[b][:], wgu[wb][:, 0, j, fcs], xsT[:, j, 0:512], j == 0, j == 7, reads=[Rwgu[wb], RxsT], pwrites=[Rpg[b]])
                        for j in range(8):
                            fw.mm(pu[b][:], wgu[wb][:, 1, j, fcs], xsT[:, j, 0:512], j == 0, j == 7, reads=[Rwgu[wb], RxsT], pwrites=[Rpu[b]])
                        fw.act(gsb[b][:], pg[b][:], AF.Silu, reads=[Rpg[b]], writes=[Rgsb[b]])
                        fw.tt("dve", hidT[:, fc, 0:512], gsb[b][:], pu[b][:], ALU.mult, reads=[Rgsb[b], Rpu[b]], pwrites=[RhidT])
                        if do_ctx:
                            for j in range(8):
                                fw.mm(pc[:, 0:32], wgu[wb][:, 0, j, fcs], xsT[:, j, 512:544], j == 0, j == 7, reads=[Rwgu[wb], RxsT], pwrites=[Rpc])
                            for j in range(8):
                                fw.mm(pc[:, 32:64], wgu[wb][:, 1, j, fcs], xsT[:, j, 512:544], j == 0, j == 7, reads=[Rwgu[wb], RxsT], pwrites=[Rpc])
                            fw.copy("act", gsc[:, 0:64], pc[:, 0:64], reads=[Rpc], writes=[Rgsc])
                            fw.act(gsc[:, 0:32], gsc[:, 0:32], AF.Silu, reads=[Rgsc], writes=[Rgsc])
                            fw.tt("dve", hidT[:, fc, 512:544], gsc[:, 0:32], gsc[:, 32:64], ALU.mult, reads=[Rgsc], pwrites=[RhidT])
                    for ti, (s, sc, ns, sl0) in enumerate(tiles):
                        for hf in range(2):
                            yb = yk % 2; yk += 1
                            cs_ = slice(hf * 512, (hf + 1) * 512)
                            for fc in range(8):
                                fw.mm(pyy[yb][0:ns, :], hidT[:, fc, sl0:sl0 + ns], wd[wb][:, fc, cs_], fc == 0, fc == 7,
                                      reads=[RhidT, Rwd[wb]], pwrites=[Rpyy[yb]])
                            if fh == 0:
                                fw.copy("act", yacc[0:ns, ti, cs_], pyy[yb][0:ns, :], reads=[Rpyy[yb]], pwrites=[Ryacc[ti]])
                            else:
                                fw.tt("dve", yacc[0:ns, ti, cs_], pyy[yb][0:ns, :], yacc[0:ns, ti, cs_], ALU.add, reads=[Rpyy[yb], Ryacc[ti]], pwrites=[Ryacc[ti]])
                for ti, (s, sc, ns, sl0) in enumerate(tiles):
                    def sc_fn(s=s, sc=sc, ns=ns, ti=ti, e=e):
                        b = sc_fn.k[0] % 2; sc_fn.k[0] += 1
                        fw.ts("dve", ysc[b][0:ns, :], yacc[0:ns, ti, :], s["valsT"][0:ns, sc, e:e + 1], None, ALU.mult,
                              reads=[Ryacc[ti], s["Rsel"]], writes=[Rysc[b]])
                        fw.dma_custom("pool", lambda en: en.indirect_dma_start(
                            out=s["acc"].ap, out_offset=bass.IndirectOffsetOnAxis(ap=s["idxT"][0:ns, sc, e:e + 1], axis=0),
                            in_=ysc[b][0:ns, :], in_offset=None, compute_op=ALU.add),
                            reads=[Rysc[b], s["Rsel"]], writes=[s["acc"].R])
                    sc_fn.k = S5_moe._k
                    deferred.append(sc_fn)
            for f in deferred:
                f()
        with fw.stage():
            L = LNCtx(fw, g, l, "ln2_w", "ln2_b")
            xt = [fw.sb(f"cxt{i}", [128, 1024], F32) for i in range(2)]; Rxt = [Res(), Res()]
            at = [fw.sb(f"cat{i}", [128, 1024], F32) for i in range(2)]; Rat = [Res(), Res()]
            k = 0
            for s in sets:
                for tt in range(s["N"] // 128):
                    b = k % 2; k += 1
                    rows = slice(tt * 128, (tt + 1) * 128)
                    fw.dma("sp", xt[b][:], s["src"].ap[rows, :], reads=[s["src"].R], writes=[Rxt[b]])
                    fw.dma("act", at[b][:], s["acc"].ap[rows, :], reads=[s["acc"].R], writes=[Rat[b]])
                    fw.tt("dve", at[b][:], at[b][:], modb[:, s["r"], 2, :], ALU.mult, reads=[Rat[b], Rmodb], writes=[Rat[b]])
                    fw.stt("dve", at[b][:], xt[b][:], ALPHA, at[b][:], ALU.mult, ALU.add, reads=[Rxt[b], Rat[b]], writes=[Rat[b]])
                    ln_tile(fw, L, at[b][:], Rat[b], s["dst"].ap[rows, :], s["dst"].R, is_output=(final and s["r"] == 0))


S5_moe._k = [0]


def pipeline(fw, g, out):
    S0_mod(fw, g)
    S1_inproj(fw, g, 0, g.x, g.ctx)
    S2_rg(fw, g, 0)
    S3_ssd(fw, g, 0)
    S4_attn(fw, g, 0, g.x, g.ctx, g.x1, g.ctx1, True)
    S5_moe(fw, g, 0, g.x1, g.ctx1, g.xa, g.ctxa, True)
    S1_inproj(fw, g, 1, g.xa, g.ctxa)
    S2_rg(fw, g, 1)
    S3_ssd(fw, g, 1)
    S4_attn(fw, g, 1, g.xa, g.ctxa, g.x1, None, False)
    S5_moe(fw, g, 1, g.x1, None, out, None, False, final=True)

def kernel(x, c, ctx, c_ctx, w_mod, b_mod, w_in, q_norm, k_norm, attn_out_norm,
           ssd_conv_w, ssd_conv_b, ssd_dt_bias, ssd_a_log, ssd_d, ssd_norm,
           rg_conv_w, rg_conv_b, rg_wa, rg_ba, rg_wx, rg_bx, rg_lambda, rg_out_norm,
           w_out, ln1_w, ln1_b, w_router, w_gate, w_up, w_down, ln2_w, ln2_b):
    f = lambda a: np.ascontiguousarray(np.asarray(a, dtype=np.float32))
    small = dict(q_norm=f(q_norm), k_norm=f(k_norm), attn_out_norm=f(attn_out_norm), ssd_conv_w=f(ssd_conv_w),
                 ssd_conv_b=f(ssd_conv_b), ssd_d=f(ssd_d), ssd_norm=f(ssd_norm), rg_conv_w=f(rg_conv_w),
                 rg_conv_b=f(rg_conv_b), rg_ba=f(rg_ba), rg_bx=f(rg_bx), rg_lambda=f(rg_lambda), rg_out_norm=f(rg_out_norm))
    shared = dict(b_mod=f(b_mod), ssd_dt_bias=f(ssd_dt_bias).reshape(2, 8),
                  ssd_a_log=f(ssd_a_log).reshape(2, 8), rg_wa=f(rg_wa), rg_wx=f(rg_wx), w_out=f(w_out),
                  ln1_w=f(ln1_w), ln1_b=f(ln1_b), w_router=f(w_router), ln2_w=f(ln2_w), ln2_b=f(ln2_b),
                  pp=np.stack([host_pp(small, l) for l in range(2)]), consts=host_consts(), rope=host_rope())
    w_mod = f(w_mod); w_in = f(w_in); w_gate = f(w_gate); w_up = f(w_up); w_down = f(w_down)
    for l in range(2):
        shared[f"w_in{l}"] = w_in[l]
        for h in range(2):
            shared[f"w_mod{l}_{h}"] = w_mod[l, h * 512:(h + 1) * 512]
        for e in range(16):
            shared[f"wg{l}_{e}"] = w_gate[l, e]
            shared[f"wu{l}_{e}"] = w_up[l, e]
            shared[f"wd{l}_{e}"] = w_down[l, e]
    x = f(x); ctx = f(ctx); c = f(c); c_ctx = f(c_ctx)
    nb = x.shape[0]
    nc = bass.Bass("TRN2", target_bir_lowering=False)
    with ExitStack() as es:
        fw = FW(nc, es)
        g = make_G(nc)
        out = DT(nc, "out", [4096, 1024], F32, kind="ExternalOutput")
        pipeline(fw, g, out)
        fw.finish()
    in_maps = []
    for b in range(nb):
        cc = np.zeros((128, 16), np.float32)
        cc[:, 0::2] = c[b].reshape(8, 128).T
        cc[:, 1::2] = c_ctx.reshape(8, 128).T
        in_maps.append(dict(shared, x=x[b], ctx=ctx[b], cc=cc))
    res = run_bass_kernel_spmd(nc, in_maps, core_ids=list(range(nb)))
    return np.stack([np.asarray(r["out"], dtype=np.float32) for r in res.results], axis=0)
```
